# Optimizing a Trainium2 kernel written in Bass

```python
import jax, jax.numpy as jnp
from jax import lax
import numpy as np

D_MODEL = 1024
BATCH = 8
SEQ = 2048
DEPTH = 2

GRID_W = 64
CTX_LEN = 256
HEAD_DIM = 64
FOURIER_GROUPS = 4
FOURIER_GROUP_CH = 64
FOURIER_W = FOURIER_GROUPS * FOURIER_GROUP_CH
G_HEADS = 8
G_KV = 2
G_GROUP = G_HEADS // G_KV
W_HEADS = 4
W_KV = 2
W_GROUP = W_HEADS // W_KV
WINDOW = 128
Q_BLOCK = 128
N_BRANCH = 3
SPLIT_SIZES = (FOURIER_W,
               G_HEADS * HEAD_DIM, G_KV * HEAD_DIM, G_KV * HEAD_DIM,
               W_HEADS * HEAD_DIM, W_KV * HEAD_DIM, W_KV * HEAD_DIM,
               N_BRANCH * D_MODEL)
IN_W = sum(SPLIT_SIZES)
ROPE_THETA = 10000.0
N_EXPERTS = 16
CAPACITY_FACTOR = 2
EXPERT_FF = D_MODEL
EPS = 1e-6
NEG_INF = -1e30

kernel_name = "hybrid_dit_fourier_gqa_window_ecmoe"


def rms_norm(x, g):
    xf = x.astype(jnp.float32)
    y = xf * lax.rsqrt(jnp.mean(jnp.square(xf), axis=-1, keepdims=True) + EPS)
    return (y * g.astype(jnp.float32)).astype(x.dtype)


def modulate(h, shift, scale):
    return h * (1 + scale[:, None, :]) + shift[:, None, :]


def heads(z, n):
    return z.reshape(z.shape[:-1] + (n, HEAD_DIM))


def split_proj(z):
    points = np.cumsum(SPLIT_SIZES)[:-1].tolist()
    return jnp.split(z, points, axis=-1)


def axial_rope_angles(n_tokens):
    rows = n_tokens // GRID_W
    r, col = jnp.meshgrid(jnp.arange(rows), jnp.arange(GRID_W), indexing="ij")
    half = HEAD_DIM // 2
    inv = ROPE_THETA ** (-jnp.arange(0, half, 2, dtype=jnp.float32) / half)
    ang = jnp.concatenate([r.reshape(-1, 1).astype(jnp.float32) * inv,
                           col.reshape(-1, 1).astype(jnp.float32) * inv], axis=-1)
    return jnp.cos(ang), jnp.sin(ang)


def apply_rope(x, cos, sin):
    xf = x.astype(jnp.float32).reshape(x.shape[:-1] + (HEAD_DIM // 2, 2))
    x0, x1 = xf[..., 0], xf[..., 1]
    cs, sn = cos[None, :, None, :], sin[None, :, None, :]
    out = jnp.stack([x0 * cs - x1 * sn, x0 * sn + x1 * cs], axis=-1)
    return out.reshape(x.shape).astype(x.dtype)


def attend(q, k, v):
    s = jnp.einsum("bqhgd,bkhd->bhgqk", q, k).astype(jnp.float32) * (HEAD_DIM ** -0.5)
    p = jax.nn.softmax(s, axis=-1).astype(v.dtype)
    return jnp.einsum("bhgqk,bkhd->bqhgd", p, v)


def global_attention(q_l, k_l, v_l, k_c, v_c):
    B, T = q_l.shape[:2]
    nb = T // Q_BLOCK
    k_all = jnp.concatenate([k_c, k_l], axis=1)
    v_all = jnp.concatenate([v_c, v_l], axis=1)
    qb = q_l.reshape(B, nb, Q_BLOCK, G_KV, G_GROUP, HEAD_DIM).transpose(1, 0, 2, 3, 4, 5)
    ob = lax.map(lambda qq: attend(qq, k_all, v_all), qb)
    return ob.transpose(1, 0, 2, 3, 4, 5).reshape(B, T, G_HEADS * HEAD_DIM)


def window_attention(q_l, k_l, v_l, k_c, v_c, sink):
    B, T = q_l.shape[:2]
    L = k_c.shape[1]
    nb = T // Q_BLOCK
    q = q_l.reshape(B, nb, Q_BLOCK, W_KV, W_GROUP, HEAD_DIM)

    def band(t):
        tp = jnp.pad(t, ((0, 0), (Q_BLOCK, Q_BLOCK), (0, 0), (0, 0)))
        tp = tp.reshape(B, nb + 2, Q_BLOCK, W_KV, HEAD_DIM)
        return jnp.concatenate([tp[:, :-2], tp[:, 1:-1], tp[:, 2:]], axis=2)

    kb, vb = band(k_l), band(v_l)
    scale = HEAD_DIM ** -0.5
    s_c = jnp.einsum("bnqhgd,bkhd->bnhgqk", q, k_c).astype(jnp.float32) * scale
    s_b = jnp.einsum("bnqhgd,bnkhd->bnhgqk", q, kb).astype(jnp.float32) * scale
    qpos = jnp.arange(Q_BLOCK)[:, None]
    kpos = jnp.arange(3 * Q_BLOCK)[None, :]
    rel = qpos - kpos + Q_BLOCK
    key_abs = (jnp.arange(nb)[:, None, None] - 1) * Q_BLOCK + kpos[None]
    valid = (jnp.abs(rel)[None] <= WINDOW) & (key_abs >= 0) & (key_abs < T)
    s_b = jnp.where(valid[None, :, None, None], s_b, NEG_INF)
    s_sink = jnp.broadcast_to(sink.astype(jnp.float32).reshape(1, 1, W_KV, W_GROUP, 1, 1),
                              s_c.shape[:-1] + (1,))
    p = jax.nn.softmax(jnp.concatenate([s_c, s_b, s_sink], axis=-1), axis=-1).astype(v_l.dtype)
    o = (jnp.einsum("bnhgqk,bkhd->bnqhgd", p[..., :L], v_c)
         + jnp.einsum("bnhgqk,bnkhd->bnqhgd", p[..., L:L + 3 * Q_BLOCK], vb))
    return o.reshape(B, T, W_HEADS * HEAD_DIM)


def sink_attention_ctx(q_c, k_c, v_c, sink):
    B, L = q_c.shape[:2]
    q = q_c.reshape(B, L, W_KV, W_GROUP, HEAD_DIM)
    s = jnp.einsum("bqhgd,bkhd->bhgqk", q, k_c).astype(jnp.float32) * (HEAD_DIM ** -0.5)
    s_sink = jnp.broadcast_to(sink.astype(jnp.float32).reshape(1, W_KV, W_GROUP, 1, 1),
                              s.shape[:-1] + (1,))
    p = jax.nn.softmax(jnp.concatenate([s, s_sink], axis=-1), axis=-1)[..., :L].astype(v_c.dtype)
    o = jnp.einsum("bhgqk,bkhd->bqhgd", p, v_c)
    return o.reshape(B, L, W_HEADS * HEAD_DIM)


def fourier_mix(u):
    B, N, _ = u.shape
    g = u.astype(jnp.float32).reshape(B, N, FOURIER_GROUPS, FOURIER_GROUP_CH)
    f = jnp.fft.fft2(g, axes=(1, 3), norm="ortho").real
    return f.reshape(B, N, FOURIER_W).astype(u.dtype)


def gated_merge(gate_cols, f_mix, o_g, o_w, w_br_fourier, w_br_global, w_br_window, w_out):
    g = jax.nn.sigmoid(gate_cols).reshape(gate_cols.shape[:-1] + (N_BRANCH, D_MODEL))
    m = (g[..., 0, :] * (f_mix @ w_br_fourier)
         + g[..., 1, :] * (o_g @ w_br_global)
         + g[..., 2, :] * (o_w @ w_br_window))
    return m @ w_out


def mixer_sublayer(h, hc, cos, sin, w_in, q_norm_g, k_norm_g, sink,
                   w_br_fourier, w_br_global, w_br_window, w_out, need_ctx):
    B, T, _ = h.shape
    L = hc.shape[1]
    lf, lgq, lgk, lgv, lwq, lwk, lwv, lgate = split_proj(h @ w_in)
    cf, cgq, cgk, cgv, cwq, cwk, cwv, cgate = split_proj(hc @ w_in)
    kc_g = rms_norm(heads(cgk, G_KV), k_norm_g)
    vc_g = heads(cgv, G_KV)
    kc_w = heads(cwk, W_KV)
    vc_w = heads(cwv, W_KV)
    q_g = apply_rope(rms_norm(heads(lgq, G_HEADS), q_norm_g), cos, sin)
    k_g = apply_rope(rms_norm(heads(lgk, G_KV), k_norm_g), cos, sin)
    o_g = global_attention(q_g, k_g, heads(lgv, G_KV), kc_g, vc_g)
    q_w = apply_rope(heads(lwq, W_HEADS), cos, sin)
    k_w = apply_rope(heads(lwk, W_KV), cos, sin)
    o_w = window_attention(q_w, k_w, heads(lwv, W_KV), kc_w, vc_w, sink)
    y = gated_merge(lgate, fourier_mix(lf), o_g, o_w,
                    w_br_fourier, w_br_global, w_br_window, w_out)
    if not need_ctx:
        return y, None
    qc_g = rms_norm(heads(cgq, G_HEADS), q_norm_g).reshape(B, L, G_KV, G_GROUP, HEAD_DIM)
    oc_g = attend(qc_g, kc_g, vc_g).reshape(B, L, G_HEADS * HEAD_DIM)
    oc_w = sink_attention_ctx(heads(cwq, W_HEADS), kc_w, vc_w, sink)
    yc = gated_merge(cgate, fourier_mix(cf), oc_g, oc_w,
                     w_br_fourier, w_br_global, w_br_window, w_out)
    return y, yc


def expert_choice_ffn(h, w_router, w_gate_e, w_up_e, w_down_e):
    B, N, _ = h.shape
    cap = CAPACITY_FACTOR * N // N_EXPERTS
    aff = jax.nn.softmax((h @ w_router).astype(jnp.float32), axis=-1)
    top_aff, top_idx = lax.top_k(aff.transpose(0, 2, 1), cap)
    bidx = jnp.arange(B)[:, None, None]
    xg = h[bidx, top_idx]
    a = jnp.einsum("becd,edf->becf", xg, w_gate_e)
    u = jnp.einsum("becd,edf->becf", xg, w_up_e)
    out = jnp.einsum("becf,efd->becd", jax.nn.silu(a) * u, w_down_e) * top_aff[..., None].astype(h.dtype)
    return jnp.zeros_like(h).at[bidx, top_idx].add(out)


def setup_inputs(seed: int = 0) -> dict:
    key = jax.random.key(seed)
    ks = jax.random.split(key, 24)
    D, F, E = D_MODEL, EXPERT_FF, N_EXPERTS
    nrm = jax.random.normal
    f32 = jnp.float32
    return {
        "x": nrm(ks[0], (BATCH, SEQ, D), f32),
        "c": nrm(ks[1], (BATCH, D), f32),
        "ctx": nrm(ks[2], (BATCH, CTX_LEN, D), f32),
        "c_ctx": nrm(ks[3], (D,), f32),
        "w_ada": nrm(ks[4], (DEPTH, D, 6 * D), f32) * (0.5 * D ** -0.5),
        "b_ada": nrm(ks[5], (DEPTH, 6 * D), f32) * 0.01,
        "norm1_g": 1.0 + 0.05 * nrm(ks[6], (DEPTH, D), f32),
        "w_in": nrm(ks[7], (DEPTH, D, IN_W), f32) * D ** -0.5,
        "q_norm_g": 1.0 + 0.05 * nrm(ks[8], (DEPTH, HEAD_DIM), f32),
        "k_norm_g": 1.0 + 0.05 * nrm(ks[9], (DEPTH, HEAD_DIM), f32),
        "sink": 0.5 * nrm(ks[10], (DEPTH, W_HEADS), f32),
        "w_br_fourier": nrm(ks[11], (DEPTH, FOURIER_W, D), f32) * FOURIER_W ** -0.5,
        "w_br_global": nrm(ks[12], (DEPTH, G_HEADS * HEAD_DIM, D), f32) * (G_HEADS * HEAD_DIM) ** -0.5,
        "w_br_window": nrm(ks[13], (DEPTH, W_HEADS * HEAD_DIM, D), f32) * (W_HEADS * HEAD_DIM) ** -0.5,
        "w_out": nrm(ks[14], (DEPTH, D, D), f32) * D ** -0.5,
        "norm2_g": 1.0 + 0.05 * nrm(ks[15], (DEPTH, D), f32),
        "w_router": nrm(ks[16], (DEPTH, D, E), f32) * D ** -0.5,
        "w_gate_e": nrm(ks[17], (DEPTH, E, D, F), f32) * D ** -0.5,
        "w_up_e": nrm(ks[18], (DEPTH, E, D, F), f32) * D ** -0.5,
        "w_down_e": nrm(ks[19], (DEPTH, E, F, D), f32) * F ** -0.5,
        "final_g": 1.0 + 0.05 * nrm(ks[20], (D,), f32),
    }


def reference(x, c, ctx, c_ctx, w_ada, b_ada, norm1_g, w_in, q_norm_g, k_norm_g, sink,
              w_br_fourier, w_br_global, w_br_window, w_out, norm2_g, w_router,
              w_gate_e, w_up_e, w_down_e, final_g):
    T = x.shape[1]
    cos, sin = axial_rope_angles(T)
    xc = ctx
    silu_c = jax.nn.silu(c)
    silu_cc = jax.nn.silu(c_ctx)[None]
    for l in range(DEPTH):
        need_ctx = l < DEPTH - 1
        mod = silu_c @ w_ada[l] + b_ada[l]
        mod_c = silu_cc @ w_ada[l] + b_ada[l]
        sh1, sc1, g1, sh2, sc2, g2 = jnp.split(mod, 6, axis=-1)
        csh1, csc1, cg1, csh2, csc2, cg2 = jnp.split(mod_c, 6, axis=-1)
        h = modulate(rms_norm(x, norm1_g[l]), sh1, sc1)
        hc = modulate(rms_norm(xc, norm1_g[l]), csh1, csc1)
        y, yc = mixer_sublayer(h, hc, cos, sin, w_in[l], q_norm_g[l], k_norm_g[l], sink[l],
                               w_br_fourier[l], w_br_global[l], w_br_window[l], w_out[l], need_ctx)
        x = x + g1[:, None, :] * y
        h = modulate(rms_norm(x, norm2_g[l]), sh2, sc2)
        x = x + g2[:, None, :] * expert_choice_ffn(h, w_router[l], w_gate_e[l], w_up_e[l], w_down_e[l])
        if need_ctx:
            xc = xc + cg1[:, None, :] * yc
            hc = modulate(rms_norm(xc, norm2_g[l]), csh2, csc2)
            xc = xc + cg2[:, None, :] * expert_choice_ffn(hc, w_router[l], w_gate_e[l], w_up_e[l], w_down_e[l])
    return rms_norm(x, final_g)
```

```python
import numpy as np
import ml_dtypes
from contextlib import ExitStack
import concourse.bass as bass
import concourse.mybir as mybir
from concourse.bass_utils import run_bass_kernel_spmd

F32 = mybir.dt.float32
F32R = mybir.dt.float32r
BF16 = mybir.dt.bfloat16
ALU = mybir.AluOpType
AF = mybir.ActivationFunctionType
AX = mybir.AxisListType
COMPUTE = ("pe", "act", "dve", "pool")


class Tok:
    __slots__ = ("name", "writers", "readers")

    def __init__(self, name="t"):
        self.name = name
        self.writers = []
        self.readers = {}


class DSem:
    def __init__(self, sem):
        self.sem = sem
        self.count = 0


class Op:
    __slots__ = ("eng", "fn", "deps", "sig", "signals", "idx", "is_dma", "dsem")

    def __init__(self, eng, fn, is_dma, dsem):
        self.eng = eng
        self.fn = fn
        self.deps = []
        self.sig = None
        self.signals = False
        self.is_dma = is_dma
        self.dsem = dsem


class Sched:
    def __init__(self, nc):
        self.nc = nc
        self.ops = []

    def _add(self, eng, fn, reads, writes, is_dma=False, dsem=None):
        op = Op(eng, fn, is_dma, dsem)
        op.idx = len(self.ops)
        deps = {}
        for t in reads:
            for w in t.writers:
                if w is op:
                    continue
                if (not w.is_dma) and (not is_dma) and w.eng == eng and eng == "pe":
                    continue
                deps[w.idx] = w
        for t in writes:
            for w in t.writers:
                if (not w.is_dma) and (not is_dma) and w.eng == eng:
                    continue
                if w.is_dma and is_dma:
                    continue
                deps[w.idx] = w
            for r in t.readers.values():
                if (not r.is_dma) and (not is_dma) and r.eng == eng:
                    continue
                if r is op:
                    continue
                deps[r.idx] = r
        op.deps = list(deps.values())
        for p in op.deps:
            p.signals = True
        for t in writes:
            if is_dma and t.writers and all(w.is_dma for w in t.writers) and not t.readers:
                t.writers = t.writers + [op]
            else:
                t.writers = [op]
            t.readers = {}
        for t in reads:
            if t in writes:
                continue
            key = ("dma", id(dsem)) if is_dma else eng
            t.readers[key] = op
        if is_dma:
            op.signals = True
        self.ops.append(op)
        return op

    def mm(self, out, lhsT, rhs, start=True, stop=True, reads=(), writes=(), **kw):
        return self._add("pe", lambda e: e.matmul(out, lhsT, rhs, start=start, stop=stop, **kw), reads, writes)

    def tr(self, out, in_, ident, reads=(), writes=()):
        return self._add("pe", lambda e: e.transpose(out, in_, ident), reads, writes)

    def act(self, out, in_, func, reads=(), writes=(), **kw):
        return self._add("act", lambda e: e.activation(out, in_, func, **kw), reads, writes)

    def ex(self, eng, name, *args, reads=(), writes=(), **kw):
        return self._add(eng, lambda e: getattr(e, name)(*args, **kw), reads, writes)

    def dve(self, name, *args, reads=(), writes=(), **kw):
        return self.ex("dve", name, *args, reads=reads, writes=writes, **kw)

    def pool(self, name, *args, reads=(), writes=(), **kw):
        return self.ex("pool", name, *args, reads=reads, writes=writes, **kw)

    def dma(self, out, in_, dsem, reads=(), writes=(), eng="sp", **kw):
        return self._add(eng, lambda e: e.dma_start(out=out, in_=in_, **kw), reads, writes, is_dma=True, dsem=dsem)

    def barrier(self):
        last = {}
        for op in self.ops:
            if (not op.is_dma) and op.fn is not None:
                last[op.eng] = op
        for o in last.values():
            o.signals = True
        for eng in ("pe", "act", "dve", "pool", "sp"):
            op = Op(eng, None, False, None)
            op.idx = len(self.ops)
            self.ops.append(op)

    def emit(self, block, sems):
        cnt = {e: 0 for e in COMPUTE}
        dcount = {}
        dobj = {}
        for op in self.ops:
            if op.fn is None:
                op.sig = (dict(cnt), dict(dcount))
                continue
            if op.is_dma:
                op.dsem.count += 16
                dcount[id(op.dsem)] = op.dsem.count
                dobj[id(op.dsem)] = op.dsem
                op.sig = (op.dsem, op.dsem.count)
            elif op.signals:
                cnt[op.eng] += 1
                op.sig = (op.eng, cnt[op.eng])
        per_eng = {e: [] for e in ("pe", "act", "dve", "pool", "sp")}
        for op in self.ops:
            per_eng[op.eng].append(op)

        def run(eng_name, e):
            known = {}
            for op in per_eng[eng_name]:
                if op.fn is None:
                    ccnt, dcnt = op.sig
                    for en, v in ccnt.items():
                        if v > 0 and known.get(en, 0) < v and en != eng_name:
                            e.wait_ge(sems[en], v)
                            known[en] = v
                    for di, v in dcnt.items():
                        if known.get(di, 0) < v:
                            e.wait_ge(dobj[di].sem, v)
                            known[di] = v
                    continue
                waits = {}
                for p in op.deps:
                    k, v = p.sig
                    kk = id(k) if isinstance(k, DSem) else k
                    if known.get(kk, 0) >= v:
                        continue
                    if kk not in waits or waits[kk][1] < v:
                        waits[kk] = (k, v)
                for kk, (k, v) in waits.items():
                    sem = k.sem if isinstance(k, DSem) else sems[k]
                    e.wait_ge(sem, v)
                    known[kk] = v
                ins = op.fn(e)
                if op.is_dma:
                    ins.then_inc(op.dsem.sem, 16)
                elif op.signals:
                    ins.then_inc(sems[op.eng], 1)
            if eng_name == "sp":
                for d in dobj.values():
                    if d.count > 0:
                        e.wait_ge(d.sem, d.count)
                for en in COMPUTE:
                    if cnt[en] > 0:
                        e.wait_ge(sems[en], cnt[en])

        @block.tensor
        def _(e):
            run("pe", e)

        @block.scalar
        def _(e):
            run("act", e)

        @block.vector
        def _(e):
            run("dve", e)

        @block.gpsimd
        def _(e):
            run("pool", e)

        @block.sync
        def _(e):
            run("sp", e)


class Ring:
    def __init__(self, items):
        self.items = items
        self.i = 0

    def next(self):
        it = self.items[self.i % len(self.items)]
        self.i += 1
        return it


D = 1024
T = 2048
L = 256
TA = T + L
NJ = TA // 128
E = 16
EPS = 1e-6
DEPTH = 2
GROUPS = [(0, 256), (256, 512), (768, 512), (1280, 512), (1792, 512)]
CH_F = [0, 1]
CH_GQ = [2, 3, 4, 5]
CH_GQS = [6, 7, 8, 9]
CH_GK, CH_GKS = 10, 11
CH_WQ = [12, 13]
CH_WQS = [14, 15]
CH_WK, CH_WKS = 16, 17
CH_GATE0 = 18
NCHUNK = 42


def build(depth=DEPTH, dbg=(), stop_after=None):
    nc = bass.Bass("TRN2", target_bir_lowering=False)
    nc.dge_precook = False
    S = Sched(nc)
    uctr = [0]

    def uniq(name):
        uctr[0] += 1
        return f"sb_{name}_{uctr[0]}"

    def din(name, shape, dt=F32):
        return nc.dram_tensor(name, list(shape), dt, kind="ExternalInput").ap()

    def dscr(name, shape, dt):
        kind = "ExternalOutput" if name in dbg else "Internal"
        return nc.dram_tensor(name, list(shape), dt, kind=kind).ap()

    xin = din("xin", [TA, D])
    ccin = din("ccin", [128, 8, 2])
    w_ada = din("w_ada", [DEPTH, D, 6 * D], F32R)
    bcol_in = din("bcol", [128, DEPTH, 48])
    n1col_in = din("n1col", [128, DEPTH, 8])
    n2col_in = din("n2col", [128, DEPTH, 8])
    w_inp = din("w_inp", [DEPTH, D, NCHUNK * 128], F32R)
    w_v = din("w_v", [DEPTH, D, 256], F32R)
    qkg_in = din("qkg", [128, DEPTH, 4])
    sink_in = din("sink", [DEPTH, 4])
    w_br = din("w_br", [DEPTH, D, D])
    w_out = din("w_out", [DEPTH, D, D])
    w_r = din("w_r", [DEPTH, D, E])
    w_ge = din("w_ge", [DEPTH, E, D, D], F32R)
    w_ue = din("w_ue", [DEPTH, E, D, D], F32R)
    w_de = din("w_de", [DEPTH, E, D, D], F32R)
    fg_in = din("final_g", [D])
    identf_in = din("identf", [128, 128])
    identb_in = din("identb", [128, 128], BF16)
    bd_in = din("bd", [128, 128], BF16)
    cos_in = din("cos", [128, T])
    sin_in = din("sin", [128, T])
    mk_in = din("mk", [128, 2, 256], BF16)
    iota_in = din("iota", [128, 256])
    pidx_in = din("pidx", [128, 2])
    selb_in = din("selb", [16, 16, 128], BF16)
    csc_in = din("csc", [128, 256], BF16)
    cs2048 = din("cs2048", [2, T, T], BF16)
    cs256 = din("cs256", [2, L, L], BF16)
    out = nc.dram_tensor("out", [T, D], F32, kind="ExternalOutput").ap()

    xs = dscr("xs", [TA, D], F32)
    GTs = dscr("GTs", [2, 128, TA], BF16)
    QgTs = dscr("QgTs", [128, NJ, 4, 128], BF16)
    KgTs = dscr("KgTs", [128, TA], BF16)
    QwTs = dscr("QwTs", [128, NJ, 2, 128], BF16)
    KwTs = dscr("KwTs", [128, TA], BF16)
    Vs = dscr("Vs", [TA, 256], BF16)
    sgTs = dscr("sgTs", [24, 128, TA], BF16)
    FTs = dscr("FTs", [2, 128, TA], BF16)
    OgTs = dscr("OgTs", [4, 128, TA], BF16)
    OwTs = dscr("OwTs", [2, 128, TA], BF16)
    dbg_aff = dscr("dbg_aff", [16, T], F32)
    dbg_mask = dscr("dbg_mask", [16, T], F32)

    with ExitStack() as ges:
        sems = {e: ges.enter_context(nc.semaphore("s_" + e)) for e in COMPUTE}
        DSP = [DSem(ges.enter_context(nc.semaphore(f"d{i}"))) for i in range(48)]
        dctr = [0]

        def newd():
            d = DSP[dctr[0] % len(DSP)]
            dctr[0] += 1
            return d

        def gsb(name, shape, dt):
            return ges.enter_context(nc.sbuf_tensor(uniq(name), list(shape), dt))

        pall = ges.enter_context(nc.psum_tensor("pall", [128, 4096], F32))
        pbanks = [pall[:, i * 512:(i + 1) * 512] for i in range(8)]
        PSI = [(pbanks[i], Tok(f"pb{i}")) for i in range(8)]
        PS_D = Ring([(pall[:, 2048:3072], Tok("pd0")), (pall[:, 3072:4096], Tok("pd1"))])
        PS = Ring(PSI)
        PS_O = Ring(PSI[0:4])
        PS_S = Ring(PSI[4:7])
        PS_T = Ring(PSI[7:8])

        identf = gsb("identf", [128, 128], F32)
        identb = gsb("identb", [128, 128], BF16)
        bd = gsb("bd", [128, 128], BF16)
        modcol = gsb("modcol", [128, DEPTH, 48, 2], F32)
        A1col = gsb("A1col", [128, DEPTH, 8, 2], F32)
        A2col = gsb("A2col", [128, DEPTH, 8, 2], F32)
        epsc = gsb("epsc", [128, 1], F32)
        tconst = Tok("const")
        tmodl = [Tok("mod0"), Tok("mod1")]
        S.dma(identf[:], identf_in, newd(), writes=[tconst])
        S.dma(identb[:], identb_in, newd(), writes=[tconst])
        S.dma(bd[:], bd_in, newd(), writes=[tconst])
        S.dve("memset", epsc[:], EPS, writes=[tconst])
        S.barrier()

        def sbring(pes, name, n, shape, dt, with_dsem=False):
            items = []
            for i in range(n):
                t = pes.enter_context(nc.sbuf_tensor(uniq(f"{name}{i}"), list(shape), dt))
                if with_dsem:
                    items.append((t, Tok(f"{name}{i}"), newd()))
                else:
                    items.append((t, Tok(f"{name}{i}")))
            return Ring(items)

        def rowform(dst, tdst, col8, tcol, tmpring):
            for half in range(2):
                pb, tpb = PS.next()
                for b in range(4):
                    kc = half * 4 + b
                    cb, tcb = tmpring.next()
                    S.dve("tensor_copy", cb[:], col8[:, kc:kc + 1].to_broadcast([128, 128]), reads=[tcol], writes=[tcb])
                    S.mm(pb[:, b * 128:(b + 1) * 128], cb[:], identf[:], reads=[tcb, tconst], writes=[tpb])
                S.dve("tensor_copy", dst[:, half * 512:(half + 1) * 512], pb[:], reads=[tpb], writes=[tdst])

        cc = gsb("cc", [128, 8, 2], F32)
        sc = gsb("sc", [128, 8, 2], F32)
        bcol = gsb("bcol", [128, DEPTH, 48], F32)
        n1col = gsb("n1col", [128, DEPTH, 8], F32)
        n2col = gsb("n2col", [128, DEPTH, 8], F32)
        tsc, tb = Tok(), Tok()

        def mod_layer(l, pes, evac):
            WA = sbring(pes, "wa", 3, [128, 8, 512], F32R, with_dsem=True)
            for n in range(12):
                wt, twt, dwt = WA.next()
                S.dma(wt[:], w_ada[l][:, n * 512:(n + 1) * 512].rearrange("(kc p) j -> p kc j", p=128), dwt,
                      writes=[twt])
                for mi in range(4):
                    m = n * 4 + mi
                    pb, tpb = PS.next()
                    for kc in range(8):
                        S.mm(pb[:, 0:2], wt[:, kc, mi * 128:(mi + 1) * 128].bitcast(F32), sc[:, kc, :],
                             start=(kc == 0), stop=(kc == 7), reads=[twt, tsc], writes=[tpb])
                    if evac == "act":
                        S.act(modcol[:, l, m, :], pb[:, 0:2], AF.Identity, bias=bcol[:, l, m:m + 1], scale=1.0,
                              reads=[tpb, tb], writes=[tmodl[l]])
                    else:
                        S.dve("tensor_scalar", modcol[:, l, m, :], pb[:, 0:2], bcol[:, l, m:m + 1], 0.0,
                              ALU.add, ALU.add, reads=[tpb, tb], writes=[tmodl[l]])

        def mod_derive(l):
            for s in range(2):
                S.dve("scalar_tensor_tensor", A1col[:, l, :, s], modcol[:, l, 8:16, s], 1.0, n1col[:, l, :],
                      ALU.add, ALU.mult, reads=[tmodl[l], tb], writes=[tmodl[l]])
                S.dve("scalar_tensor_tensor", A2col[:, l, :, s], modcol[:, l, 32:40, s], 1.0, n2col[:, l, :],
                      ALU.add, ALU.mult, reads=[tmodl[l], tb], writes=[tmodl[l]])

        def phase_mod():
            with ExitStack() as pes:
                tcc = Tok()
                S.dma(cc[:], ccin, newd(), writes=[tcc])
                S.dma(bcol[:], bcol_in, newd(), writes=[tb])
                S.dma(n1col[:], n1col_in, newd(), writes=[tb])
                S.dma(n2col[:], n2col_in, newd(), writes=[tb])
                S.act(sc[:], cc[:], AF.Silu, reads=[tcc], writes=[tsc])
                mod_layer(0, pes, "dve")
                mod_derive(0)
                S.barrier()

        def phase_inproj(l):
            need_ctx = l < depth - 1
            with ExitStack() as pes:
                def sb(name, shape, dt):
                    return pes.enter_context(nc.sbuf_tensor(uniq(name), list(shape), dt))
                hT = sb("hT", [128, 8, TA], F32R)
                thT = [Tok(f"hT{j}") for j in range(NJ)]
                ss = sb("ss", [128, NJ], F32)
                rstd = sb("rstd", [128, NJ], F32)
                cosT = sb("cosT", [128, T], F32)
                sinT = sb("sinT", [128, T], F32)
                qkg = sb("qkg", [128, DEPTH, 4], F32)
                wv = sb("wv", [128, 8, 256], F32R)
                XT = sbring(pes, "xt", 3, [128, D], F32, with_dsem=True)
                JK = sbring(pes, "jk", 2, [128, D], F32)
                XN = sbring(pes, "xn", 2, [128, D], F32)
                WR = sbring(pes, "wr", 6, [128, 8, 128], F32R, with_dsem=True)
                OS = sbring(pes, "os", 8, [128, 512], BF16, with_dsem=True)
                SQ = sbring(pes, "sq", 3, [128, 512], BF16)
                RS = sbring(pes, "rs", 3, [128, 512], F32)
                T1 = sbring(pes, "t1", 3, [128, 512], F32)
                T2 = sbring(pes, "t2", 3, [128, 512], F32)
                tl = Tok("loads")
                tss = Tok("ss")
                S.dma(cosT[:], cos_in, newd(), writes=[tl])
                S.dma(sinT[:], sin_in, newd(), writes=[tl])
                S.dma(qkg[:], qkg_in, newd(), writes=[tl])
                S.dma(wv[:], w_v[l].rearrange("(kc p) j -> p kc j", p=128), newd(), writes=[tl])
                S.dve("memset", ss[:], 0.0, writes=[tss])
                eps64 = sb("eps64", [128, 1], F32)
                S.dve("memset", eps64[:], 64 * EPS, writes=[tl])
                for j in range(NJ):
                    s = 1 if j < 2 else 0
                    xt, txt, dxt = XT.next()
                    S.dma(xt[:], (xin if l == 0 else xs)[j * 128:(j + 1) * 128, :], dxt, writes=[txt])
                    jk, tjk = JK.next()
                    tsj = Tok()
                    S.act(jk[:], xt[:], AF.Square, accum_out=ss[:, j:j + 1], reads=[txt, tss], writes=[tjk, tsj])
                    S.act(rstd[:, j:j + 1], ss[:, j:j + 1], AF.Ln, bias=epsc[:, 0:1], scale=1.0 / D,
                          reads=[tsj, tconst], writes=[tsj])
                    S.act(rstd[:, j:j + 1], rstd[:, j:j + 1], AF.Exp, scale=-0.5, reads=[tsj], writes=[tsj])
                    xn, txn = XN.next()
                    S.act(xn[:], xt[:], AF.Copy, scale=rstd[:, j:j + 1], reads=[txt, tsj], writes=[txn])
                    for half in range(2):
                        pb, tpb = PS.next()
                        for b in range(4):
                            kc = half * 4 + b
                            S.tr(pb[:, b * 128:(b + 1) * 128], xn[:, kc * 128:(kc + 1) * 128], identf[:],
                                 reads=[txn, tconst], writes=[tpb])
                        for b in range(4):
                            kc = half * 4 + b
                            S.dve("tensor_scalar", hT[:, kc, j * 128:(j + 1) * 128], pb[:, b * 128:(b + 1) * 128],
                                  A1col[:, l, kc, s:s + 1], modcol[:, l, kc, s:s + 1], ALU.mult, ALU.add,
                                  reads=[tpb, tmodl[l]], writes=[thT[j]])

                def gtoks(goff, gn):
                    return thT[goff // 128:(goff + gn) // 128]

                worder = list(CH_F) + [CH_GATE0 + gc for gc in range(24)]
                for ci in range(4):
                    worder += [CH_GQ[ci], CH_GQS[ci]]
                worder += [CH_GK, CH_GKS]
                for ci in range(2):
                    worder += [CH_WQ[ci], CH_WQS[ci]]
                worder += [CH_WK, CH_WKS]
                wq = []
                wstate = [0, 0]

                def load_w(c):
                    while wstate[0] < min(len(worder), wstate[1] + 5):
                        cn = worder[wstate[0]]
                        wt, twt, dwt = WR.next()
                        S.dma(wt[:], w_inp[l][:, cn * 128:(cn + 1) * 128].rearrange("(kc p) j -> p kc j", p=128), dwt,
                              writes=[twt])
                        wq.append((cn, wt, twt))
                        wstate[0] += 1
                    cn, wt, twt = wq.pop(0)
                    assert cn == c, (cn, c)
                    wstate[1] += 1
                    return wt, twt

                def proj(wt, twt, goff, gn):
                    pb, tpb = PS.next()
                    for kc in range(8):
                        S.mm(pb[:, :gn], wt[:, kc, :], hT[:, kc, goff:goff + gn], start=(kc == 0), stop=(kc == 7),
                             reads=[twt] + gtoks(goff, gn), writes=[tpb])
                    return pb, tpb

                def groups_for(ctx_needed):
                    return [g for gi, g in enumerate(GROUPS) if gi > 0 or ctx_needed]

                for ci, c in enumerate(CH_F):
                    wt, twt = load_w(c)
                    for goff, gn in groups_for(need_ctx):
                        pb, tpb = proj(wt, twt, goff, gn)
                        st, tst, dst_ = OS.next()
                        S.dve("tensor_copy", st[:, :gn], pb[:, :gn], reads=[tpb], writes=[tst])
                        S.dma(GTs[ci][:, goff:goff + gn], st[:, :gn], dst_, reads=[tst])
                for gc in range(24):
                    wt, twt = load_w(CH_GATE0 + gc)
                    for goff, gn in groups_for(need_ctx):
                        pb, tpb = proj(wt, twt, goff, gn)
                        st, tst, dst_ = OS.next()
                        S.act(st[:, :gn], pb[:, :gn], AF.Sigmoid, reads=[tpb], writes=[tst])
                        S.dma(sgTs[gc][:, goff:goff + gn], st[:, :gn], dst_, reads=[tst])

                def qk_chunk(c, cs, gi, dst_fn, norm, ctx_needed):
                    wt, twt = load_w(c)
                    ws, tws = load_w(cs)
                    def stage1(goff, gn):
                        latent = goff >= 256
                        pz, tpz = proj(wt, twt, goff, gn)
                        pzs, tpzs = proj(ws, tws, goff, gn) if latent else (None, None)
                        sq, tsq = (None, None)
                        if norm:
                            sq, tsq = SQ.next()
                            S.act(sq[:, :gn], pz[:, :gn], AF.Square, reads=[tpz], writes=[tsq])
                        return (goff, gn, latent, pz, tpz, pzs, tpzs, sq, tsq)

                    def stage2(goff, gn, latent, pz, tpz, pzs, tpzs, sq, tsq):
                        lo = goff - 256
                        st, tst, dst_ = OS.next()
                        if norm:
                            pss, tpss = PS.next()
                            S.mm(pss[:, :gn], bd[:], sq[:, :gn], reads=[tsq, tconst], writes=[tpss])
                            rs, trs = RS.next()
                            S.act(rs[:, :gn], pss[:, :gn], AF.Ln, bias=eps64[:, 0:1], scale=1.0, reads=[tpss, tl],
                                  writes=[trs])
                            S.act(rs[:, :gn], rs[:, :gn], AF.Exp, scale=-0.5, reads=[trs], writes=[trs])
                            t1, tt1 = T1.next()
                            S.dve("scalar_tensor_tensor", t1[:, :gn], pz[:, :gn], qkg[:, l, gi:gi + 1], rs[:, :gn],
                                  ALU.mult, ALU.mult, reads=[tpz, trs, tl], writes=[tt1])
                            if latent:
                                t2, tt2 = T2.next()
                                S.dve("scalar_tensor_tensor", t2[:, :gn], pzs[:, :gn], qkg[:, l, gi + 1:gi + 2],
                                      rs[:, :gn], ALU.mult, ALU.mult, reads=[tpzs, trs, tl], writes=[tt2])
                                S.pool("tensor_tensor", t1[:, :gn], t1[:, :gn], cosT[:, lo:lo + gn], ALU.mult,
                                       reads=[tt1, tl], writes=[tt1])
                                S.pool("tensor_tensor", t2[:, :gn], t2[:, :gn], sinT[:, lo:lo + gn], ALU.mult,
                                       reads=[tt2, tl], writes=[tt2])
                                S.pool("tensor_tensor", st[:, :gn], t1[:, :gn], t2[:, :gn], ALU.add,
                                       reads=[tt1, tt2], writes=[tst])
                            else:
                                S.dve("tensor_copy", st[:, :gn], t1[:, :gn], reads=[tt1], writes=[tst])
                        else:
                            if latent:
                                t1, tt1 = T1.next()
                                t2, tt2 = T2.next()
                                S.dve("tensor_tensor", t1[:, :gn], pz[:, :gn], cosT[:, lo:lo + gn], ALU.mult,
                                      reads=[tpz, tl], writes=[tt1])
                                S.dve("tensor_tensor", t2[:, :gn], pzs[:, :gn], sinT[:, lo:lo + gn], ALU.mult,
                                      reads=[tpzs, tl], writes=[tt2])
                                S.pool("tensor_tensor", st[:, :gn], t1[:, :gn], t2[:, :gn], ALU.add,
                                       reads=[tt1, tt2], writes=[tst])
                            else:
                                S.dve("tensor_copy", st[:, :gn], pz[:, :gn], reads=[tpz], writes=[tst])
                        S.dma(dst_fn(goff, gn), dst_fn(None, gn, st), dst_, reads=[tst])

                    prev = None
                    for goff, gn in groups_for(ctx_needed):
                        cur = stage1(goff, gn)
                        if prev is not None:
                            stage2(*prev)
                        prev = cur
                    stage2(*prev)

                def qdst(scr, c):
                    def f(goff, gn, st=None):
                        if st is not None:
                            return st[:, :gn].rearrange("p (j q) -> p j q", q=128)
                        return scr[:, goff // 128:(goff + gn) // 128, c, :]
                    return f

                def kdst(scr):
                    def f(goff, gn, st=None):
                        if st is not None:
                            return st[:, :gn]
                        return scr[:, goff:goff + gn]
                    return f
                for ci in range(4):
                    qk_chunk(CH_GQ[ci], CH_GQS[ci], 0, qdst(QgTs, ci), True, need_ctx)
                qk_chunk(CH_GK, CH_GKS, 2, kdst(KgTs), True, True)
                for ci in range(2):
                    qk_chunk(CH_WQ[ci], CH_WQS[ci], None, qdst(QwTs, ci), False, need_ctx)
                qk_chunk(CH_WK, CH_WKS, None, kdst(KwTs), False, True)
                for j in range(NJ):
                    pb, tpb = PS.next()
                    for kc in range(8):
                        S.mm(pb[:, :256], hT[:, kc, j * 128:(j + 1) * 128], wv[:, kc, :], start=(kc == 0),
                             stop=(kc == 7), reads=[thT[j], tl], writes=[tpb])
                    st, tst, dst_ = OS.next()
                    S.dve("tensor_copy", st[:, :256], pb[:, :256], reads=[tpb], writes=[tst])
                    S.dma(Vs[j * 128:(j + 1) * 128, :], st[:, :256], dst_, reads=[tst])
                S.barrier()

        def phase_fourier(l):
            need_ctx = l < depth - 1
            with ExitStack() as pes:
                def sb(name, shape, dt):
                    return pes.enter_context(nc.sbuf_tensor(uniq(name), list(shape), dt))
                GT = sb("GT", [128, 2, TA], BF16)
                csc = sb("csc", [128, 256], BF16)
                AB = sb("AB", [128, NJ, 2, 256], BF16)
                ct256 = sb("ct256", [128, 2, 2, 256], BF16)
                CT = sbring(pes, "ct", 4, [128, 2, 16, 512], BF16, with_dsem=True)
                OS = sbring(pes, "osf", 3, [128, 512], BF16, with_dsem=True)
                tl = Tok()
                tAB = [Tok() for _ in range(NJ)]
                j0 = 0 if need_ctx else 2
                for c in range(2):
                    S.dma(GT[:, c, j0 * 128:], GTs[c][:, j0 * 128:], newd(), writes=[tl])
                S.dma(csc[:], csc_in, newd(), writes=[tl])
                if need_ctx:
                    for cs in range(2):
                        S.dma(ct256[:, cs], cs256[cs].rearrange("(j p) k -> p j k", p=128), newd(), writes=[tl])
                for j in range(j0, NJ):
                    for c in range(2):
                        pb, tpb = PS.next()
                        S.mm(pb[:, :256], GT[:, c, j * 128:(j + 1) * 128], csc[:], reads=[tl], writes=[tpb])
                        if c == 0:
                            S.dve("tensor_copy", AB[:, j, c, :], pb[:, :256], reads=[tpb], writes=[tAB[j]])
                        else:
                            S.act(AB[:, j, c, :], pb[:, :256], AF.Copy, reads=[tpb], writes=[tAB[j]])
                cts = []
                for kg in range(4):
                    ct, tct, dct = CT.next()
                    for cs in range(2):
                        S.dma(ct[:, cs], cs2048[cs][:, kg * 512:(kg + 1) * 512].rearrange("(j p) k -> p j k", p=128),
                              dct, writes=[tct])
                    cts.append((ct, tct))
                for kg in range(4):
                    ct, tct = cts[kg]
                    for c in range(2):
                        pb, tpb = PS.next()
                        for j in range(16):
                            for cs in range(2):
                                S.mm(pb[:], AB[:, 2 + j, c, cs * 128:(cs + 1) * 128], ct[:, cs, j, :],
                                     start=(j == 0 and cs == 0), stop=(j == 15 and cs == 1),
                                     reads=[tAB[2 + j], tct], writes=[tpb])
                        st, tst, dst_ = OS.next()
                        S.dve("tensor_copy", st[:], pb[:], reads=[tpb], writes=[tst])
                        S.dma(FTs[c][:, 256 + kg * 512:256 + (kg + 1) * 512], st[:], dst_, reads=[tst])
                if need_ctx:
                    for c in range(2):
                        pb, tpb = PS.next()
                        for j in range(2):
                            for cs in range(2):
                                S.mm(pb[:, :256], AB[:, j, c, cs * 128:(cs + 1) * 128], ct256[:, cs, j, :],
                                     start=(j == 0 and cs == 0), stop=(j == 1 and cs == 1),
                                     reads=[tAB[j], tl], writes=[tpb])
                        st, tst, dst_ = OS.next()
                        S.dve("tensor_copy", st[:, :256], pb[:, :256], reads=[tpb], writes=[tst])
                        S.dma(FTs[c][:, 0:256], st[:, :256], dst_, reads=[tst])
                S.barrier()

        def phase_attn(l):
            need_ctx = l < depth - 1
            with ExitStack() as pes:
                def sb(name, shape, dt):
                    return pes.enter_context(nc.sbuf_tensor(uniq(name), list(shape), dt))
                Kg = sb("Kg", [128, TA], BF16)
                Kw = sb("Kw", [128, TA], BF16)
                Qg = [sb(f"Qg{g}", [128, NJ, 4, 128], BF16) for g in range(2)]
                Qw = [sb(f"Qw{g}", [128, NJ, 2, 128], BF16) for g in range(2)]
                Va = sb("Va", [128, NJ, 4, 65], BF16)
                OgT = sb("OgT", [128, 4, TA], BF16)
                OwT = sb("OwT", [128, 2, TA], BF16)
                mk = sb("mk", [128, 2, 256], BF16)
                esk = sb("esk", [128, 4], F32)
                EX = sbring(pes, "ex", 3, [128, 1024], BF16)
                OTM = sbring(pes, "otm", 2, [128, 512], BF16)
                RC = sbring(pes, "rc", 4, [128, 4], F32)
                tl, tV, tsk = Tok(), Tok(), Tok()
                tVd = Tok()
                tO = Tok()
                j0 = 0 if need_ctx else 2
                S.dma(Kg[:], KgTs, newd(), writes=[tl])
                S.dma(Kw[:], KwTs, newd(), writes=[tl])
                tq0 = Tok()
                for g in range(2):
                    o = 1 - g
                    S.pool("memset", Qg[g][o * 64:(o + 1) * 64], 0.0, writes=[tq0])
                    S.pool("memset", Qw[g][o * 64:(o + 1) * 64], 0.0, writes=[tq0])
                    S.dma(Qg[g][g * 64:(g + 1) * 64, j0:], QgTs[g * 64:(g + 1) * 64, j0:], newd(), writes=[tl])
                    S.dma(Qw[g][g * 64:(g + 1) * 64, j0:], QwTs[g * 64:(g + 1) * 64, j0:], newd(), writes=[tl])
                S.dma(mk[:], mk_in, newd(), writes=[tl])
                S.dma(esk[:], sink_in[l].partition_broadcast(128), newd(), writes=[tsk])
                S.act(esk[:], esk[:], AF.Exp, reads=[tsk], writes=[tsk])
                S.pool("memset", Va[:], 1.0, writes=[tV])
                dva = newd()
                for g4 in range(4):
                    S.dma(Va[:, :, g4, 0:64], Vs[:, g4 * 64:(g4 + 1) * 64].rearrange("(j p) d -> p j d", p=128), dva,
                          reads=[tV], writes=[tVd])
                tV = tVd

                fin = []

                def attend(jq, keys, Q, K, nh, voff, scale, masks, sink, OT):
                    nq = nh * 128
                    otm, totm = OTM.next()
                    pos = [PS_O.next() for _ in range(2)]

                    def issue_s(ki):
                        kc = keys[ki]
                        dt_, tdt = PS_D.next()
                        for g in range(2):
                            S.mm(dt_[:, g * nq:(g + 1) * nq], K[:, kc * 128:(kc + 1) * 128],
                                 Q[g][:, jq, :, :].rearrange("p c q -> p (c q)"),
                                 reads=[tl, tq0], writes=[tdt])
                        return dt_, tdt
                    pend = issue_s(0)
                    for ki, kc in enumerate(keys):
                        dt_, tdt = pend
                        if ki + 1 < len(keys):
                            pend = issue_s(ki + 1)
                        if ki == 0:
                            while fin:
                                fin.pop(0)()
                        ex, tex = EX.next()
                        S.act(ex[:, :2 * nq], dt_[:, :2 * nq], AF.Exp, scale=scale, reads=[tdt], writes=[tex])
                        if kc in masks:
                            for g in range(2):
                                S.pool("tensor_tensor", ex[:, g * nq:(g + 1) * nq], ex[:, g * nq:(g + 1) * nq],
                                       mk[:, masks[kc], :], ALU.mult, reads=[tex, tl], writes=[tex])
                        for g in range(2):
                            po, tpo = pos[g]
                            for h in range(nh):
                                S.mm(po[:, h * 65:(h + 1) * 65], ex[:, g * nq + h * 128:g * nq + (h + 1) * 128],
                                     Va[:, kc, voff + g, :], start=(ki == 0 and h == 0), stop=(ki == len(keys) - 1),
                                     reads=[tex, tV], writes=[tpo], skip_group_check=True)
                    for g in range(2):
                        po, tpo = pos[g]
                        rc, trc = RC.next()
                        po3 = po[:, 0:nh * 65].rearrange("p (h d) -> p h d", d=65)
                        if sink:
                            S.dve("tensor_tensor", rc[:, 0:nh], po3[:, :, 64], esk[:, g * 2:g * 2 + nh], ALU.add,
                                  reads=[tpo, tsk], writes=[trc])
                            S.dve("reciprocal", rc[:, 0:nh], rc[:, 0:nh], reads=[trc], writes=[trc])
                        else:
                            S.dve("reciprocal", rc[:, 0:nh], po3[:, :, 64], reads=[tpo], writes=[trc])
                        for h in range(nh):
                            S.dve("tensor_scalar", otm[:, (g * nh + h) * 64:(g * nh + h + 1) * 64],
                                  po[:, h * 65:h * 65 + 64], rc[:, h:h + 1], 0.0, ALU.mult, ALU.add,
                                  reads=[tpo, trc], writes=[totm])
                    nblk = 2 * nh * 64 // 128

                    def finish():
                        pt, tpt = pos[0]
                        ptb = pt.bitcast(BF16)
                        for c in range(nblk):
                            S.tr(ptb[:, c * 128:(c + 1) * 128], otm[:, c * 128:(c + 1) * 128], identb[:],
                                 reads=[totm, tconst], writes=[tpt])
                        S.dve("tensor_copy", OT[:, :, jq * 128:(jq + 1) * 128],
                              ptb[:, 0:nblk * 128].rearrange("p (c q) -> p c q", c=nblk),
                              reads=[tpt], writes=[tO])
                    fin.append(finish)

                for jq in range(j0, NJ):
                    if jq < 2:
                        gkeys = [0, 1]
                        wkeys = [0, 1]
                        masks = {}
                    else:
                        gkeys = list(range(NJ))
                        wkeys = [0, 1]
                        masks = {}
                        if jq - 1 >= 2:
                            wkeys.append(jq - 1)
                            masks[jq - 1] = 0
                        wkeys.append(jq)
                        if jq + 1 < NJ:
                            wkeys.append(jq + 1)
                            masks[jq + 1] = 1
                    attend(jq, gkeys, Qg, Kg, 4, 0, 8.0, {}, False, OgT)
                    attend(jq, wkeys, Qw, Kw, 2, 2, 0.125, masks, True, OwT)
                while fin:
                    fin.pop(0)()
                for c in range(4):
                    S.dma(OgTs[c][:, j0 * 128:], OgT[:, c, j0 * 128:], newd(), reads=[tO])
                for c in range(2):
                    S.dma(OwTs[c][:, j0 * 128:], OwT[:, c, j0 * 128:], newd(), reads=[tO])
                S.barrier()

        def phase_merge(l):
            need_ctx = l < depth - 1
            with ExitStack() as pes:
                def sb(name, shape, dt):
                    return pes.enter_context(nc.sbuf_tensor(uniq(name), list(shape), dt))
                wbr = sb("wbr", [128, 8, D], BF16)
                wo = sb("wo", [128, 8, D], BF16)
                g1row = [sb(f"g1row{s}", [128, D], F32) for s in range(2)]
                tg1 = Tok()
                WST = sbring(pes, "wst", 2, [128, 8, 256], F32, with_dsem=True)
                CB = sbring(pes, "cb", 2, [128, 128], F32)
                tw = Tok()
                for (wsrc, wdst) in ((w_br, wbr), (w_out, wo)):
                    for qq in range(4):
                        st, tst, dst_ = WST.next()
                        S.dma(st[:], wsrc[l][:, qq * 256:(qq + 1) * 256].rearrange("(kc p) j -> p kc j", p=128),
                              dst_, writes=[tst])
                        S.dve("tensor_copy", wdst[:, :, qq * 256:(qq + 1) * 256], st[:], reads=[tst], writes=[tw])
                for s in range(2 if need_ctx else 1):
                    rowform(g1row[s], tg1, modcol[:, l, 16:24, s], tmodl[l], CB)
                fT = sbring(pes, "fT", 2, [128, 8, 512], BF16, with_dsem=True)
                sg = sbring(pes, "sg", 2, [128, 24, 512], BF16, with_dsem=True)
                mT = sbring(pes, "mT", 2, [128, 8, 512], BF16)
                TT = sbring(pes, "tt", 4, [128, 512], F32)
                XT = sbring(pes, "xt4", 8, [128, D], F32, with_dsem=True)
                XO = sbring(pes, "xo4", 3, [128, D], F32, with_dsem=True)
                act_groups = [(gi, g) for gi, g in enumerate(GROUPS) if gi > 0 or need_ctx]
                loaded = {}

                def issue_loads(ai):
                    gi, (goff, gn) = act_groups[ai]
                    f_, tf, df = fT.next()
                    for c in range(2):
                        S.dma(f_[:, c, :gn], FTs[c][:, goff:goff + gn], df, writes=[tf])
                    for c in range(4):
                        S.dma(f_[:, 2 + c, :gn], OgTs[c][:, goff:goff + gn], df, writes=[tf])
                    for c in range(2):
                        S.dma(f_[:, 6 + c, :gn], OwTs[c][:, goff:goff + gn], df, writes=[tf])
                    sg_, tsg, dsg = sg.next()
                    S.dma(sg_[:, :, :gn], sgTs[:, :, goff:goff + gn].rearrange("c p t -> p c t"), dsg, writes=[tsg])
                    xl = []
                    for tcn in range(gn // 128):
                        j = goff // 128 + tcn
                        xt, txt, dxt = XT.next()
                        S.dma(xt[:], (xin if l == 0 else xs)[j * 128:(j + 1) * 128, :], dxt, writes=[txt])
                        xl.append((xt, txt))
                    loaded[ai] = (f_, tf, sg_, tsg, xl)
                issue_loads(0)
                for ai, (gi, (goff, gn)) in enumerate(act_groups):
                    s = 1 if gi == 0 else 0
                    if ai + 1 < len(act_groups):
                        issue_loads(ai + 1)
                    f_, tf, sg_, tsg, xl = loaded.pop(ai)
                    m_, tm_ = mT.next()
                    for d in range(8):
                        tts = []
                        for b, kcs in enumerate(([0, 1], [2, 3, 4, 5], [6, 7])):
                            pb, tpb = PS.next()
                            for ki, kc in enumerate(kcs):
                                S.mm(pb[:, :gn], wbr[:, kc, d * 128:(d + 1) * 128], f_[:, kc, :gn],
                                     start=(ki == 0), stop=(ki == len(kcs) - 1), reads=[tw, tf], writes=[tpb])
                            tt, ttt = TT.next()
                            S.dve("tensor_tensor", tt[:, :gn], pb[:, :gn], sg_[:, b * 8 + d, :gn], ALU.mult,
                                  reads=[tpb, tsg], writes=[ttt])
                            tts.append((tt, ttt))
                        S.pool("tensor_tensor", tts[0][0][:, :gn], tts[0][0][:, :gn], tts[1][0][:, :gn], ALU.add,
                               reads=[tts[0][1], tts[1][1]], writes=[tts[0][1]])
                        S.dve("tensor_tensor", m_[:, d, :gn], tts[0][0][:, :gn], tts[2][0][:, :gn], ALU.add,
                              reads=[tts[0][1], tts[2][1]], writes=[tm_])
                    for tcn in range(gn // 128):
                        j = goff // 128 + tcn
                        xt, txt = xl[tcn]
                        xo, txo, dxo = XO.next()
                        for half in range(2):
                            pb, tpb = PS.next()
                            for kc in range(8):
                                S.mm(pb[:], m_[:, kc, tcn * 128:(tcn + 1) * 128], wo[:, kc, half * 512:(half + 1) * 512],
                                     start=(kc == 0), stop=(kc == 7), reads=[tm_, tw], writes=[tpb])
                            tt, ttt = TT.next()
                            S.dve("tensor_tensor", tt[:], pb[:], g1row[s][:, half * 512:(half + 1) * 512], ALU.mult,
                                  reads=[tpb, tg1], writes=[ttt])
                            S.dve("tensor_tensor", xo[:, half * 512:(half + 1) * 512], tt[:],
                                  xt[:, half * 512:(half + 1) * 512], ALU.add, reads=[ttt, txt], writes=[txo])
                        S.dma(xs[j * 128:(j + 1) * 128, :], xo[:], dxo, reads=[txo])
                S.barrier()

        def phase_moe(l, sets, dbgout=False):
            ns = len(sets)
            CT = sum(st_[2] for st_ in sets)
            offs = [sum(st_[2] for st_ in sets[:i]) for i in range(ns)]
            with ExitStack() as pes:
                def sb(name, shape, dt):
                    return pes.enter_context(nc.sbuf_tensor(uniq(name), list(shape), dt))
                iota = sb("iota", [128, 256], F32)
                pidx = sb("pidx", [128, 2], F32)
                selb = sb("selb", [16, 16, 128], BF16)
                tl = Tok()
                S.dma(iota[:], iota_in, newd(), writes=[tl])
                S.dma(pidx[:], pidx_in, newd(), writes=[tl])
                S.dma(selb[:], selb_in, newd(), writes=[tl])
                P_ = []
                for si, (j0, nch, cap, s) in enumerate(sets):
                    N = nch * 128
                    d_ = dict(j0=j0, nch=nch, cap=cap, s=s, N=N, pc=min(cap, 128), ncc=max(1, cap // 128),
                              tgs=[(o, min(512, N - o)) for o in range(0, N, 512)], off=offs[si])
                    d_["h2"] = sb(f"h2_{si}", [128, nch, D], BF16)
                    d_["th2"] = [Tok() for _ in range(nch)]
                    d_["slotT"] = sb(f"slotT{si}", [16, N], BF16)
                    d_["wT"] = sb(f"wT{si}", [16, N], BF16)
                    d_["slot_tm"] = sb(f"slot_tm{si}", [128, nch, 16], F32)
                    d_["mask_tm"] = sb(f"mask_tm{si}", [128, nch, 16], F32)
                    d_["g2row"] = sb(f"g2row{si}", [128, D], F32)
                    d_["trt"] = Tok()
                    d_["tg2"] = Tok()
                    d_["tx"] = [Tok() for _ in range(nch)]
                    P_.append(d_)
                for si, d_ in enumerate(P_):
                    j0, nch, cap, s, N = d_["j0"], d_["nch"], d_["cap"], d_["s"], d_["N"]
                    h2, th2, trt = d_["h2"], d_["th2"], d_["trt"]
                    with ExitStack() as res:
                        def rsb(name, shape, dt):
                            return res.enter_context(nc.sbuf_tensor(uniq(name), list(shape), dt))
                        A2row = rsb("A2row", [128, D], F32)
                        sh2row = rsb("sh2row", [128, D], F32)
                        wr = rsb("wr", [128, 8, E], F32)
                        aff_tm = rsb("aff_tm", [128, nch, 16], F32)
                        affT = rsb("affT", [16, N], F32)
                        work = rsb("work", [16, N], F32)
                        maskT = rsb("maskT", [16, N], F32)
                        zer = rsb("zer", [16, N], F32)
                        ss = rsb("ss5", [128, nch], F32)
                        rstd = rsb("rstd5", [128, nch], F32)
                        mx8 = rsb("mx8", [16, 8], F32)
                        CB = sbring(res, "cb5", 2, [128, 128], F32)
                        XT = sbring(res, "xt5", 4, [128, D], F32, with_dsem=True)
                        JK = sbring(res, "jk5", 2, [128, D], F32)
                        HF = sbring(res, "hf5", 3, [128, D], F32)
                        HT = sbring(res, "ht5", 4, [128, 8, 128], F32)
                        SM = sbring(res, "sm5", 3, [128, 20], F32)
                        ET = sbring(res, "et5", 3, [128, 16], F32)
                        trow, tss, taff, tlr = Tok(), Tok(), Tok(), Tok()
                        S.dma(wr[:], w_r[l].rearrange("(kc p) e -> p kc e", p=128), newd(), writes=[tlr])
                        rowform(A2row, trow, A2col[:, l, :, s], tmodl[l], CB)
                        rowform(sh2row, trow, modcol[:, l, 24:32, s], tmodl[l], CB)
                        rowform(d_["g2row"], d_["tg2"], modcol[:, l, 40:48, s], tmodl[l], CB)
                        S.dve("memset", ss[:], 0.0, writes=[tss])
                        S.pool("memset", zer[:], 0.0, writes=[trt])
                        st_x, st_hf, st_pb, st_ht = {}, {}, {}, {}

                        def s_stats(jj):
                            j = j0 + jj
                            xt, txt, dxt = XT.next()
                            S.dma(xt[:], xs[j * 128:(j + 1) * 128, :], dxt, writes=[txt])
                            jk, tjk = JK.next()
                            tsj = Tok()
                            S.act(jk[:], xt[:], AF.Square, accum_out=ss[:, jj:jj + 1], reads=[txt, tss],
                                  writes=[tjk, tsj])
                            S.act(rstd[:, jj:jj + 1], ss[:, jj:jj + 1], AF.Ln, bias=epsc[:, 0:1], scale=1.0 / D,
                                  reads=[tsj, tconst], writes=[tsj])
                            S.act(rstd[:, jj:jj + 1], rstd[:, jj:jj + 1], AF.Exp, scale=-0.5, reads=[tsj],
                                  writes=[tsj])
                            st_x[jj] = (xt, txt, tsj)

                        def s_mod(jj):
                            xt, txt, tsj = st_x.pop(jj)
                            hf, thf = HF.next()
                            S.dve("scalar_tensor_tensor", hf[:], xt[:], rstd[:, jj:jj + 1], A2row[:], ALU.mult,
                                  ALU.mult, reads=[txt, tsj, trow], writes=[thf])
                            S.dve("tensor_tensor", hf[:], hf[:], sh2row[:], ALU.add, reads=[thf, trow], writes=[thf])
                            S.act(h2[:, jj, :], hf[:], AF.Copy, reads=[thf], writes=[th2[jj]])
                            pbs_ = []
                            for half in range(2):
                                pb, tpb = PS.next()
                                for b in range(4):
                                    kc = half * 4 + b
                                    S.tr(pb[:, b * 128:(b + 1) * 128], hf[:, kc * 128:(kc + 1) * 128], identf[:],
                                         reads=[thf, tconst], writes=[tpb])
                                pbs_.append((pb, tpb))
                            st_pb[jj] = pbs_

                        def s_evac(jj):
                            ht, tht = HT.next()
                            for half, (pb, tpb) in enumerate(st_pb.pop(jj)):
                                S.dve("tensor_copy", ht[:, half * 4:(half + 1) * 4, :],
                                      pb[:].rearrange("p (b q) -> p b q", b=4), reads=[tpb], writes=[tht])
                            st_ht[jj] = (ht, tht)

                        def s_rmm(jj):
                            ht, tht = st_ht.pop(jj)
                            pl, tpl = PS.next()
                            for kc in range(8):
                                S.mm(pl[:, 0:16], ht[:, kc, :], wr[:, kc, :], start=(kc == 0), stop=(kc == 7),
                                     reads=[tht, tlr], writes=[tpl])
                            return pl, tpl

                        def s_softmax(jj, pl, tpl):
                            sm, tsm = SM.next()
                            et, tet = ET.next()
                            S.dve("reduce_max", sm[:, 0:1], pl[:, 0:16], axis=AX.X, reads=[tpl], writes=[tsm])
                            S.dve("tensor_scalar", sm[:, 1:2], sm[:, 0:1], -1.0, 0.0, ALU.mult, ALU.add, reads=[tsm],
                                  writes=[tsm])
                            S.dve("memset", sm[:, 2:3], 0.0, writes=[tsm])
                            S.act(et[:], pl[:, 0:16], AF.Exp, bias=sm[:, 1:2], scale=1.0, accum_out=sm[:, 2:3],
                                  reads=[tpl, tsm], writes=[tet, tsm])
                            return sm, tsm, et, tet

                        def s_softmax2(jj, sm, tsm, et, tet):
                            S.dve("reciprocal", sm[:, 3:4], sm[:, 2:3], reads=[tsm], writes=[tsm])
                            S.dve("tensor_scalar", aff_tm[:, jj, :], et[:], sm[:, 3:4], 0.0, ALU.mult, ALU.add,
                                  reads=[tet, tsm], writes=[taff])

                        for i in range(-2, nch + 1):
                            if 0 <= i + 2 < nch:
                                s_stats(i + 2)
                            plt = s_rmm(i - 1) if 0 <= i - 1 < nch else None
                            if 0 <= i + 1 < nch:
                                s_mod(i + 1)
                            smt = s_softmax(i - 1, *plt) if plt is not None else None
                            if 0 <= i < nch:
                                s_evac(i)
                            if smt is not None:
                                s_softmax2(i - 1, *smt)
                        if si == 0 and l + 1 < depth:
                            mod_layer(l + 1, res, "act")
                        for tg0, tgn in d_["tgs"]:
                            pb, tpb = PS.next()
                            for q in range(tgn // 128):
                                jj = tg0 // 128 + q
                                S.tr(pb[0:16, q * 128:(q + 1) * 128], aff_tm[:, jj, :], identf[:],
                                     reads=[taff, tconst], writes=[tpb])
                            S.dve("tensor_copy", affT[:, tg0:tg0 + tgn], pb[0:16, :tgn], reads=[tpb], writes=[trt])
                        S.act(work[:], affT[:], AF.Copy, reads=[trt], writes=[trt])
                        for it in range(cap // 8):
                            S.dve("max", mx8[:], work[:], reads=[trt], writes=[trt])
                            S.dve("match_replace", work[:], mx8[:], work[:], 0.0, reads=[trt], writes=[trt])
                        S.dve("tensor_tensor", maskT[:], affT[:], work[:], ALU.subtract, reads=[trt], writes=[trt])
                        S.dve("tensor_scalar", maskT[:], maskT[:], 0.0, 0.0, ALU.is_gt, ALU.add, reads=[trt],
                              writes=[trt])
                        S.dve("tensor_tensor", d_["wT"][:], maskT[:], affT[:], ALU.mult, reads=[trt], writes=[trt])
                        S.dve("tensor_tensor_scan", work[:], maskT[:], zer[:], 0.0, ALU.add, ALU.add, reads=[trt],
                              writes=[trt])
                        S.dve("tensor_tensor", work[:], work[:], maskT[:], ALU.subtract, reads=[trt], writes=[trt])
                        S.dve("tensor_copy", d_["slotT"][:], work[:], reads=[trt], writes=[trt])
                        if dbgout and si == 0:
                            S.dma(dbg_aff[:, 0:N], affT[:], newd(), reads=[trt])
                            S.dma(dbg_mask[:, 0:N], maskT[:], newd(), reads=[trt])
                        for (src, dstt) in ((work, d_["slot_tm"]), (maskT, d_["mask_tm"])):
                            pb, tpb = PS.next()
                            for jj in range(nch):
                                S.tr(pb[:, jj * 16:(jj + 1) * 16], src[:, jj * 128:(jj + 1) * 128],
                                     identf[0:16, 0:16], reads=[trt, tconst], writes=[tpb])
                            S.dve("tensor_copy", dstt[:], pb[:, 0:nch * 16].rearrange("p (j e) -> p j e", e=16),
                                  reads=[tpb], writes=[trt])
                        if si == 0 and l + 1 < depth:
                            mod_derive(l + 1)
                        S.barrier()
                NPAT = 4 if ns > 1 else 5
                for d_ in P_:
                    d_["PP"] = sbring(pes, "pp", 1, [128, d_["nch"], d_["cap"]], BF16)
                    d_["PAT"] = [(sb(f"pat{i}", [128, d_["ncc"], d_["N"]], BF16), Tok()) for i in range(NPAT)]
                    d_["OE"] = [(sb(f"oe{i}", [128, d_["ncc"], D], BF16), Tok()) for i in range(4)]
                xg = sb("xg", [128, 8, CT], F32R)
                txg = Tok()
                actT = sb("actT", [128, 8, CT], F32R)
                tact = Tok()
                WE = sbring(pes, "we", 5, [128, 8, 256], F32R, with_dsem=True)
                WB = sbring(pes, "wb", 2, [128, 512], BF16)
                SA = sbring(pes, "sa", 2, [128, CT], F32)
                fuse_final = (l == depth - 1)
                if fuse_final:
                    fgrow = sb("fgrow6", [128, D], F32)
                    jkf = sb("jkf6", [128, D], F32)
                    ssf = sb("ssf6", [128, 16], F32)
                    rstdf = sb("rstdf6", [128, 16], F32)
                    tfg, tssf = Tok(), Tok()
                    S.dma(fgrow[:], fg_in.partition_broadcast(128), newd(), writes=[tfg])
                    S.dve("memset", ssf[:], 0.0, writes=[tssf])
                XT2 = sbring(pes, "xt6", 4 if ns > 1 else 5, [128, D], F32, with_dsem=True)
                XPF = 3 if ns > 1 else 4

                def load_we(wsrc, e, q):
                    wt, twt, dwt = WE.next()
                    S.dma(wt[:], wsrc[l][e][:, q * 256:(q + 1) * 256].rearrange("(kc p) j -> p kc j", p=128), dwt,
                          writes=[twt])
                    return wt, twt

                def build_pp(e):
                    for d_ in P_:
                        nch, cap, trt = d_["nch"], d_["cap"], d_["trt"]
                        pp, tpp = d_["PP"].next()
                        d_["pp"] = (pp, tpp)
                        for jj in range(nch):
                            S.ex("dve", "tensor_scalar", pp[:, jj, :], iota[:, 0:cap], d_["slot_tm"][:, jj, e:e + 1],
                                 d_["mask_tm"][:, jj, e:e + 1], ALU.is_equal, ALU.mult, reads=[trt, tl],
                                 writes=[tpp])

                def build_pat(e, el):
                    for d_ in P_:
                        trt = d_["trt"]
                        pat, tpat = d_["PAT"][e % NPAT]
                        for tg0, tgn in d_["tgs"]:
                            pbs, tpbs = PS.next()
                            S.mm(pbs[:, :tgn], selb[:, e, :], d_["slotT"][:, tg0:tg0 + tgn], reads=[trt, tl],
                                 writes=[tpbs])
                            pbw, tpbw = PS.next()
                            S.mm(pbw[:, :tgn], selb[:, e, :], d_["wT"][:, tg0:tg0 + tgn], reads=[trt, tl],
                                 writes=[tpbw])
                            wb, twb = WB.next()
                            S.act(wb[:, :tgn], pbw[:, :tgn], AF.Copy, reads=[tpbw], writes=[twb])
                            for cc in range(d_["ncc"]):
                                S.dve("scalar_tensor_tensor", pat[:, cc, tg0:tg0 + tgn], pbs[:, :tgn],
                                      pidx[:, cc:cc + 1], wb[:, :tgn], ALU.is_equal, ALU.mult,
                                      reads=[tpbs, twb, tl], writes=[tpat])

                build_pp(0)
                build_pat(0, 0)
                for grp in range(4):
                    for el in range(4):
                        e = grp * 4 + el
                        for d in range(8):
                            for d_ in P_:
                                nch, cap, off = d_["nch"], d_["cap"], d_["off"]
                                pp, tpp = d_["pp"]
                                pb, tpb = PS.next()
                                for jj in range(nch):
                                    S.mm(pb[:, :cap], d_["h2"][:, jj, d * 128:(d + 1) * 128], pp[:, jj, :],
                                         start=(jj == 0), stop=(jj == nch - 1), reads=[d_["th2"][jj], tpp],
                                         writes=[tpb])
                                if d % 2 == 0:
                                    S.dve("tensor_copy", xg[:, d, off:off + cap], pb[:, :cap], reads=[tpb],
                                          writes=[txg])
                                else:
                                    S.act(xg[:, d, off:off + cap], pb[:, :cap], AF.Copy, reads=[tpb], writes=[txg])
                        if e + 1 < E:
                            if el < 3 or NPAT == 5:
                                build_pat(e + 1, el + 1)
                            build_pp(e + 1)
                        for q in range(4):
                            wg_, twg = load_we(w_ge, e, q)
                            wu_, twu = load_we(w_ue, e, q)
                            for fl in range(2):
                                f = q * 2 + fl
                                pa, tpa = PS.next()
                                for kc in range(8):
                                    S.mm(pa[:, :CT], wg_[:, kc, fl * 128:(fl + 1) * 128], xg[:, kc, :],
                                         start=(kc == 0), stop=(kc == 7), reads=[twg, txg], writes=[tpa])
                                pu, tpu = PS.next()
                                for kc in range(8):
                                    S.mm(pu[:, :CT], wu_[:, kc, fl * 128:(fl + 1) * 128], xg[:, kc, :],
                                         start=(kc == 0), stop=(kc == 7), reads=[twu, txg], writes=[tpu])
                                sa, tsa = SA.next()
                                S.act(sa[:, :CT], pa[:, :CT], AF.Silu, reads=[tpa], writes=[tsa])
                                S.dve("tensor_tensor", actT[:, f, :], sa[:, :CT], pu[:, :CT], ALU.mult,
                                      reads=[tsa, tpu], writes=[tact])
                        for q in range(4):
                            wd_, twd = load_we(w_de, e, q)
                            ci = 0
                            for d_ in P_:
                                oe, toe = d_["OE"][el]
                                pc = d_["pc"]
                                for cc in range(d_["ncc"]):
                                    c0 = d_["off"] + cc * 128
                                    pb, tpb = PS.next()
                                    for f in range(8):
                                        S.mm(pb[:pc, :256], actT[:, f, c0:c0 + pc], wd_[:, f, :], start=(f == 0),
                                             stop=(f == 7), reads=[tact, twd], writes=[tpb])
                                    S.dve("tensor_tensor", oe[:pc, cc, q * 256:(q + 1) * 256], pb[:pc, :256],
                                          d_["g2row"][:pc, q * 256:(q + 1) * 256], ALU.mult,
                                          reads=[tpb, d_["tg2"]], writes=[toe])
                                    ci += 1
                    for d_ in P_:
                        pc, ncc = d_["pc"], d_["ncc"]
                        xq = []
                        nxt = [0]
                        for jj in range(d_["nch"]):
                            while nxt[0] < min(d_["nch"], jj + XPF):
                                jn = nxt[0]
                                xt_, txt_, dxt_ = XT2.next()
                                S.dma(xt_[:], xs[(d_["j0"] + jn) * 128:(d_["j0"] + jn + 1) * 128, :], dxt_,
                                      reads=[d_["tx"][jn]], writes=[txt_])
                                xq.append((xt_, txt_, dxt_))
                                nxt[0] += 1
                            j = d_["j0"] + jj
                            xt, txt, dxt = xq.pop(0)
                            for half in range(2):
                                pb, tpb = PS.next()
                                n_mm = 4 * ncc
                                i_mm = 0
                                for el in range(4):
                                    for cc in range(ncc):
                                        S.mm(pb[:], d_["PAT"][(grp * 4 + el) % NPAT][0][:pc, cc, jj * 128:(jj + 1) * 128],
                                             d_["OE"][el][0][:pc, cc, half * 512:(half + 1) * 512],
                                             start=(i_mm == 0), stop=(i_mm == n_mm - 1),
                                             reads=[d_["PAT"][(grp * 4 + el) % NPAT][1], d_["OE"][el][1]], writes=[tpb])
                                        i_mm += 1
                                S.dve("tensor_tensor", xt[:, half * 512:(half + 1) * 512], pb[:],
                                      xt[:, half * 512:(half + 1) * 512], ALU.add, reads=[tpb, txt], writes=[txt])
                            if fuse_final and grp == 3 and d_["s"] == 0:
                                tsj = Tok()
                                S.act(jkf[:], xt[:], AF.Square, accum_out=ssf[:, jj:jj + 1], reads=[txt, tssf],
                                      writes=[tsj])
                                S.act(rstdf[:, jj:jj + 1], ssf[:, jj:jj + 1], AF.Ln, bias=epsc[:, 0:1],
                                      scale=1.0 / D, reads=[tsj, tconst], writes=[tsj])
                                S.act(rstdf[:, jj:jj + 1], rstdf[:, jj:jj + 1], AF.Exp, scale=-0.5, reads=[tsj],
                                      writes=[tsj])
                                S.dve("scalar_tensor_tensor", xt[:], xt[:], rstdf[:, jj:jj + 1], fgrow[:], ALU.mult,
                                      ALU.mult, reads=[txt, tsj, tfg], writes=[txt])
                                S.dma(out[jj * 128:(jj + 1) * 128, :], xt[:], dxt, reads=[txt], writes=[d_["tx"][jj]])
                            else:
                                S.dma(xs[j * 128:(j + 1) * 128, :], xt[:], dxt, reads=[txt], writes=[d_["tx"][jj]])
                    if grp < 3 and NPAT == 4:
                        build_pat(grp * 4 + 4, 0)
                S.barrier()

        def phase_final():
            with ExitStack() as pes:
                def sb(name, shape, dt):
                    return pes.enter_context(nc.sbuf_tensor(uniq(name), list(shape), dt))
                fgrow = sb("fgrow", [128, D], F32)
                ss = sb("ss7", [128, 16], F32)
                rstd = sb("rstd7", [128, 16], F32)
                XT = sbring(pes, "xt7", 3, [128, D], F32, with_dsem=True)
                JK = sbring(pes, "jk7", 2, [128, D], F32)
                XO = sbring(pes, "xo7", 3, [128, D], F32, with_dsem=True)
                tl, tss = Tok(), Tok()
                S.dma(fgrow[:], fg_in.partition_broadcast(128), newd(), writes=[tl])
                S.dve("memset", ss[:], 0.0, writes=[tss])
                for jj in range(16):
                    j = 2 + jj
                    xt, txt, dxt = XT.next()
                    S.dma(xt[:], xs[j * 128:(j + 1) * 128, :], dxt, writes=[txt])
                    jk, tjk = JK.next()
                    tsj = Tok()
                    S.act(jk[:], xt[:], AF.Square, accum_out=ss[:, jj:jj + 1], reads=[txt, tss], writes=[tjk, tsj])
                    S.act(rstd[:, jj:jj + 1], ss[:, jj:jj + 1], AF.Ln, bias=epsc[:, 0:1], scale=1.0 / D,
                          reads=[tsj, tconst], writes=[tsj])
                    S.act(rstd[:, jj:jj + 1], rstd[:, jj:jj + 1], AF.Exp, scale=-0.5, reads=[tsj], writes=[tsj])
                    xo, txo, dxo = XO.next()
                    S.dve("scalar_tensor_tensor", xo[:], xt[:], rstd[:, jj:jj + 1], fgrow[:], ALU.mult, ALU.mult,
                          reads=[txt, tsj, tl], writes=[txo])
                    S.dma(out[jj * 128:(jj + 1) * 128, :], xo[:], dxo, reads=[txo])
                S.barrier()

        phases = []
        phases.append(("mod", phase_mod))
        for l in range(depth):
            need_ctx = l < depth - 1
            phases.append((f"inproj{l}", lambda l=l: phase_inproj(l)))
            phases.append((f"fourier{l}", lambda l=l: phase_fourier(l)))
            phases.append((f"attn{l}", lambda l=l: phase_attn(l)))
            phases.append((f"merge{l}", lambda l=l: phase_merge(l)))
            sets = [(2, 16, 256, 0)] + ([(0, 2, 32, 1)] if need_ctx else [])
            phases.append((f"moe{l}", lambda l=l, sets=sets: phase_moe(l, sets, dbgout=("dbg_aff" in dbg and l == 0))))
        for name, fn in phases:
            fn()
            if stop_after == name:
                break
        with nc.Block() as block:
            S.emit(block, sems)
    return nc


def _perm_cols():
    def heads(base, hs, swap):
        cols = []
        for h in hs:
            for d in range(64):
                cols.append(base + h * 64 + (d ^ 1 if swap else d))
        return cols
    cols = list(range(0, 256))
    GQ0, GK0, WQ0, WK0 = 256, 768, 1024, 1280
    for swap in (False, True):
        for c in range(4):
            cols += heads(GQ0, [c, 4 + c], swap)
    for swap in (False, True):
        cols += heads(GK0, [0, 1], swap)
    for swap in (False, True):
        cols += heads(WQ0, [0, 2], swap)
        cols += heads(WQ0, [1, 3], swap)
    for swap in (False, True):
        cols += heads(WK0, [0, 1], swap)
    cols += list(range(1536, 4608))
    return np.array(cols, dtype=np.int64)


def _consts():
    bf = ml_dtypes.bfloat16
    c = {}
    c["identf"] = np.eye(128, dtype=np.float32)
    c["identb"] = np.eye(128, dtype=np.float32).astype(bf)
    bdm = np.zeros((128, 128), np.float32)
    bdm[:64, :64] = 1
    bdm[64:, 64:] = 1
    c["bd"] = bdm.astype(bf)
    t = np.arange(T)
    r, col = t // 64, t % 64
    half = 32
    inv = 10000.0 ** (-np.arange(0, half, 2, dtype=np.float32) / half)
    ang = np.concatenate([r[:, None].astype(np.float32) * inv, col[:, None].astype(np.float32) * inv], axis=-1)
    cosv, sinv = np.cos(ang), np.sin(ang)
    cosT = np.zeros((128, T), np.float32)
    sinT = np.zeros((128, T), np.float32)
    for p in range(128):
        d = p % 64
        i = d // 2
        cosT[p] = cosv[:, i]
        sinT[p] = -sinv[:, i] if d % 2 == 0 else sinv[:, i]
    c["cos"], c["sin"] = cosT, sinT
    kj = np.arange(128)[:, None]
    qi = np.arange(128)[None, :]
    m0 = (kj >= qi).astype(np.float32)
    m1 = (kj <= qi).astype(np.float32)
    mk = np.zeros((128, 2, 256), np.float32)
    mk[:, 0, :128] = m0
    mk[:, 0, 128:] = m0
    mk[:, 1, :128] = m1
    mk[:, 1, 128:] = m1
    c["mk"] = mk.astype(bf)
    c["iota"] = np.tile(np.arange(256, dtype=np.float32)[None, :], (128, 1))
    c["pidx"] = np.stack([np.arange(128, dtype=np.float32), np.arange(128, dtype=np.float32) + 128], axis=1)
    sel = np.zeros((16, 16, 128), np.float32)
    for e in range(16):
        sel[e, e, :] = 1
    c["selb"] = sel.astype(bf)
    cc_, mm_ = np.meshgrid(np.arange(64), np.arange(64), indexing="ij")
    Cc = np.cos(2 * np.pi * cc_ * mm_ / 64) / 8.0
    Sc = np.sin(2 * np.pi * cc_ * mm_ / 64) / 8.0
    csc = np.zeros((128, 256), np.float64)
    for gblk in range(2):
        csc[gblk * 64:(gblk + 1) * 64, gblk * 64:(gblk + 1) * 64] = Cc
        csc[gblk * 64:(gblk + 1) * 64, 128 + gblk * 64:128 + (gblk + 1) * 64] = Sc
    c["csc"] = csc.astype(np.float32).astype(bf)

    def dft(N):
        n = np.arange(N)
        kn = (n[:, None] * n[None, :]) % N
        a = 2 * np.pi * kn / N
        return np.stack([np.cos(a) / np.sqrt(N), -np.sin(a) / np.sqrt(N)]).astype(np.float32).astype(bf)
    c["cs2048"] = dft(T)
    c["cs256"] = dft(L)
    return c


_CACHE = {}


def kernel(x, c, ctx, c_ctx, w_ada, b_ada, norm1_g, w_in, q_norm_g, k_norm_g, sink, w_br_fourier, w_br_global,
           w_br_window, w_out, norm2_g, w_router, w_gate_e, w_up_e, w_down_e, final_g, _dbg=(), _stop=None):
    f32 = np.float32
    x = np.asarray(x, f32)
    B = x.shape[0]
    key = (tuple(_dbg), _stop)
    if key not in _CACHE:
        _CACHE[key] = build(dbg=tuple(_dbg), stop_after=_stop)
    nc = _CACHE[key]
    consts = _consts()
    perm = _perm_cols()
    w_in = np.asarray(w_in, f32)
    w_inp = np.ascontiguousarray(w_in[:, :, perm])
    w_v = np.ascontiguousarray(np.concatenate([w_in[:, :, 896:1024], w_in[:, :, 1408:1536]], axis=2))
    b_ada = np.asarray(b_ada, f32)
    bcol = np.ascontiguousarray(b_ada.reshape(DEPTH, 48, 128).transpose(2, 0, 1))
    n1col = np.ascontiguousarray(np.asarray(norm1_g, f32).reshape(DEPTH, 8, 128).transpose(2, 0, 1))
    n2col = np.ascontiguousarray(np.asarray(norm2_g, f32).reshape(DEPTH, 8, 128).transpose(2, 0, 1))
    qg = np.asarray(q_norm_g, f32)
    kg = np.asarray(k_norm_g, f32)
    sw = np.arange(64) ^ 1
    qkg = np.zeros((128, DEPTH, 4), f32)
    for l in range(DEPTH):
        qkg[:, l, 0] = np.tile(qg[l], 2)
        qkg[:, l, 1] = np.tile(qg[l][sw], 2)
        qkg[:, l, 2] = np.tile(kg[l], 2)
        qkg[:, l, 3] = np.tile(kg[l][sw], 2)
    w_brc = np.ascontiguousarray(np.concatenate([np.asarray(w_br_fourier, f32), np.asarray(w_br_global, f32),
                                                 np.asarray(w_br_window, f32)], axis=1))
    shared = dict(
        w_ada=np.asarray(w_ada, f32), bcol=bcol, n1col=n1col, n2col=n2col, w_inp=w_inp, w_v=w_v, qkg=qkg,
        sink=np.asarray(sink, f32), w_br=w_brc, w_out=np.asarray(w_out, f32), w_r=np.asarray(w_router, f32),
        w_ge=np.asarray(w_gate_e, f32), w_ue=np.asarray(w_up_e, f32), w_de=np.asarray(w_down_e, f32),
        final_g=np.asarray(final_g, f32), **consts)
    c = np.asarray(c, f32)
    ctx = np.asarray(ctx, f32)
    c_ctx = np.asarray(c_ctx, f32)
    in_maps = []
    for b in range(B):
        cc = np.stack([c[b].reshape(8, 128).T, c_ctx.reshape(8, 128).T], axis=2)
        m = dict(shared)
        m["xin"] = np.ascontiguousarray(np.concatenate([ctx[b], x[b]], axis=0))
        m["ccin"] = np.ascontiguousarray(cc)
        in_maps.append(m)
    res = run_bass_kernel_spmd(nc, in_maps, core_ids=list(range(B)))
    if _dbg:
        return res.results
    return np.stack([r["out"] for r in res.results], axis=0).astype(f32)
```

```python
import numpy as np
import ml_dtypes
from contextlib import ExitStack
import concourse.bass as bass
import concourse.mybir as mybir
from concourse.bass_utils import run_bass_kernel_spmd

F32 = mybir.dt.float32
F32R = mybir.dt.float32r
BF16 = mybir.dt.bfloat16
ALU = mybir.AluOpType
AF = mybir.ActivationFunctionType
AX = mybir.AxisListType
COMPUTE = ("pe", "act", "dve", "pool")


class Tok:
    __slots__ = ("name", "writers", "readers")

    def __init__(self, name="t"):
        self.name = name
        self.writers = []
        self.readers = {}


class DSem:
    def __init__(self, sem):
        self.sem = sem
        self.count = 0


class Op:
    __slots__ = ("eng", "fn", "deps", "sig", "signals", "idx", "is_dma", "dsem")

    def __init__(self, eng, fn, is_dma, dsem):
        self.eng = eng
        self.fn = fn
        self.deps = []
        self.sig = None
        self.signals = False
        self.is_dma = is_dma
        self.dsem = dsem


class Sched:
    def __init__(self, nc):
        self.nc = nc
        self.ops = []

    def _add(self, eng, fn, reads, writes, is_dma=False, dsem=None):
        op = Op(eng, fn, is_dma, dsem)
        op.idx = len(self.ops)
        deps = {}
        for t in reads:
            for w in t.writers:
                if w is op:
                    continue
                if (not w.is_dma) and (not is_dma) and w.eng == eng and eng == "pe":
                    continue
                deps[w.idx] = w
        for t in writes:
            for w in t.writers:
                if (not w.is_dma) and (not is_dma) and w.eng == eng:
                    continue
                if w.is_dma and is_dma:
                    continue
                deps[w.idx] = w
            for r in t.readers.values():
                if (not r.is_dma) and (not is_dma) and r.eng == eng:
                    continue
                if r is op:
                    continue
                deps[r.idx] = r
        op.deps = list(deps.values())
        for p in op.deps:
            p.signals = True
        for t in writes:
            if is_dma and t.writers and all(w.is_dma for w in t.writers) and not t.readers:
                t.writers = t.writers + [op]
            else:
                t.writers = [op]
            t.readers = {}
        for t in reads:
            if t in writes:
                continue
            key = ("dma", id(dsem)) if is_dma else eng
            t.readers[key] = op
        if is_dma:
            op.signals = True
        self.ops.append(op)
        return op

    def mm(self, out, lhsT, rhs, start=True, stop=True, reads=(), writes=(), **kw):
        return self._add("pe", lambda e: e.matmul(out, lhsT, rhs, start=start, stop=stop, **kw), reads, writes)

    def tr(self, out, in_, ident, reads=(), writes=()):
        return self._add("pe", lambda e: e.transpose(out, in_, ident), reads, writes)

    def act(self, out, in_, func, reads=(), writes=(), **kw):
        return self._add("act", lambda e: e.activation(out, in_, func, **kw), reads, writes)

    def ex(self, eng, name, *args, reads=(), writes=(), **kw):
        return self._add(eng, lambda e: getattr(e, name)(*args, **kw), reads, writes)

    def dve(self, name, *args, reads=(), writes=(), **kw):
        return self.ex("dve", name, *args, reads=reads, writes=writes, **kw)

    def pool(self, name, *args, reads=(), writes=(), **kw):
        return self.ex("pool", name, *args, reads=reads, writes=writes, **kw)

    def dma(self, out, in_, dsem, reads=(), writes=(), eng="sp", **kw):
        return self._add(eng, lambda e: e.dma_start(out=out, in_=in_, **kw), reads, writes, is_dma=True, dsem=dsem)

    def barrier(self):
        last = {}
        for op in self.ops:
            if (not op.is_dma) and op.fn is not None:
                last[op.eng] = op
        for o in last.values():
            o.signals = True
        for eng in ("pe", "act", "dve", "pool", "sp"):
            op = Op(eng, None, False, None)
            op.idx = len(self.ops)
            self.ops.append(op)

    def emit(self, block, sems):
        cnt = {e: 0 for e in COMPUTE}
        dcount = {}
        dobj = {}
        for op in self.ops:
            if op.fn is None:
                op.sig = (dict(cnt), dict(dcount))
                continue
            if op.is_dma:
                op.dsem.count += 16
                dcount[id(op.dsem)] = op.dsem.count
                dobj[id(op.dsem)] = op.dsem
                op.sig = (op.dsem, op.dsem.count)
            elif op.signals:
                cnt[op.eng] += 1
                op.sig = (op.eng, cnt[op.eng])
        per_eng = {e: [] for e in ("pe", "act", "dve", "pool", "sp")}
        for op in self.ops:
            per_eng[op.eng].append(op)

        def run(eng_name, e):
            known = {}
            for op in per_eng[eng_name]:
                if op.fn is None:
                    ccnt, dcnt = op.sig
                    for en, v in ccnt.items():
                        if v > 0 and known.get(en, 0) < v and en != eng_name:
                            e.wait_ge(sems[en], v)
                            known[en] = v
                    for di, v in dcnt.items():
                        if known.get(di, 0) < v:
                            e.wait_ge(dobj[di].sem, v)
                            known[di] = v
                    continue
                waits = {}
                for p in op.deps:
                    k, v = p.sig
                    kk = id(k) if isinstance(k, DSem) else k
                    if known.get(kk, 0) >= v:
                        continue
                    if kk not in waits or waits[kk][1] < v:
                        waits[kk] = (k, v)
                for kk, (k, v) in waits.items():
                    sem = k.sem if isinstance(k, DSem) else sems[k]
                    e.wait_ge(sem, v)
                    known[kk] = v
                ins = op.fn(e)
                if op.is_dma:
                    ins.then_inc(op.dsem.sem, 16)
                elif op.signals:
                    ins.then_inc(sems[op.eng], 1)
            if eng_name == "sp":
                for d in dobj.values():
                    if d.count > 0:
                        e.wait_ge(d.sem, d.count)
                for en in COMPUTE:
                    if cnt[en] > 0:
                        e.wait_ge(sems[en], cnt[en])

        @block.tensor
        def _(e):
            run("pe", e)

        @block.scalar
        def _(e):
            run("act", e)

        @block.vector
        def _(e):
            run("dve", e)

        @block.gpsimd
        def _(e):
            run("pool", e)

        @block.sync
        def _(e):
            run("sp", e)


class Ring:
    def __init__(self, items):
        self.items = items
        self.i = 0

    def next(self):
        it = self.items[self.i % len(self.items)]
        self.i += 1
        return it


D = 1024
T = 2048
L = 256
TA = T + L
NJ = TA // 128
E = 16
EPS = 1e-6
DEPTH = 2
GROUPS = [(0, 256), (256, 512), (768, 512), (1280, 512), (1792, 512)]
CH_F = [0, 1]
CH_GQ = [2, 3, 4, 5]
CH_GQS = [6, 7, 8, 9]
CH_GK, CH_GKS = 10, 11
CH_WQ = [12, 13]
CH_WQS = [14, 15]
CH_WK, CH_WKS = 16, 17
CH_GATE0 = 18
NCHUNK = 42


def build(depth=DEPTH, dbg=(), stop_after=None):
    nc = bass.Bass("TRN2", target_bir_lowering=False)
    nc.dge_precook = False
    S = Sched(nc)
    uctr = [0]

    def uniq(name):
        uctr[0] += 1
        return f"sb_{name}_{uctr[0]}"

    def din(name, shape, dt=F32):
        return nc.dram_tensor(name, list(shape), dt, kind="ExternalInput").ap()

    def dscr(name, shape, dt):
        kind = "ExternalOutput" if name in dbg else "Internal"
        return nc.dram_tensor(name, list(shape), dt, kind=kind).ap()

    xin = din("xin", [TA, D])
    ccin = din("ccin", [128, 8, 2])
    w_ada = din("w_ada", [DEPTH, D, 6 * D], F32R)
    bcol_in = din("bcol", [128, DEPTH, 48])
    n1col_in = din("n1col", [128, DEPTH, 8])
    n2col_in = din("n2col", [128, DEPTH, 8])
    w_inp = din("w_inp", [DEPTH, D, NCHUNK * 128], F32R)
    w_v = din("w_v", [DEPTH, D, 256], F32R)
    qkg_in = din("qkg", [128, DEPTH, 4])
    sink_in = din("sink", [DEPTH, 4])
    w_br = din("w_br", [DEPTH, D, D])
    w_out = din("w_out", [DEPTH, D, D])
    w_r = din("w_r", [DEPTH, D, E])
    w_ge = din("w_ge", [DEPTH, E, D, D], F32R)
    w_ue = din("w_ue", [DEPTH, E, D, D], F32R)
    w_de = din("w_de", [DEPTH, E, D, D], F32R)
    fg_in = din("final_g", [D])
    identf_in = din("identf", [128, 128])
    identb_in = din("identb", [128, 128], BF16)
    bd_in = din("bd", [128, 128], BF16)
    cos_in = din("cos", [128, T])
    sin_in = din("sin", [128, T])
    mk_in = din("mk", [128, 2, 256], BF16)
    iota_in = din("iota", [128, 256])
    pidx_in = din("pidx", [128, 2])
    selb_in = din("selb", [16, 16, 128], BF16)
    csc_in = din("csc", [128, 256], BF16)
    cs2048 = din("cs2048", [2, T, T], BF16)
    cs256 = din("cs256", [2, L, L], BF16)
    out = nc.dram_tensor("out", [T, D], F32, kind="ExternalOutput").ap()

    xs = dscr("xs", [TA, D], F32)
    GTs = dscr("GTs", [2, 128, TA], BF16)
    QgTs = dscr("QgTs", [128, NJ, 4, 128], BF16)
    KgTs = dscr("KgTs", [128, TA], BF16)
    QwTs = dscr("QwTs", [128, NJ, 2, 128], BF16)
    KwTs = dscr("KwTs", [128, TA], BF16)
    Vs = dscr("Vs", [TA, 256], BF16)
    sgTs = dscr("sgTs", [24, 128, TA], BF16)
    FTs = dscr("FTs", [2, 128, TA], BF16)
    OgTs = dscr("OgTs", [4, 128, TA], BF16)
    OwTs = dscr("OwTs", [2, 128, TA], BF16)
    dbg_aff = dscr("dbg_aff", [16, T], F32)
    dbg_mask = dscr("dbg_mask", [16, T], F32)

    with ExitStack() as ges:
        sems = {e: ges.enter_context(nc.semaphore("s_" + e)) for e in COMPUTE}
        DSP = [DSem(ges.enter_context(nc.semaphore(f"d{i}"))) for i in range(48)]
        dctr = [0]

        def newd():
            d = DSP[dctr[0] % len(DSP)]
            dctr[0] += 1
            return d

        def gsb(name, shape, dt):
            return ges.enter_context(nc.sbuf_tensor(uniq(name), list(shape), dt))

        pall = ges.enter_context(nc.psum_tensor("pall", [128, 4096], F32))
        pbanks = [pall[:, i * 512:(i + 1) * 512] for i in range(8)]
        PSI = [(pbanks[i], Tok(f"pb{i}")) for i in range(8)]
        PS_D = Ring([(pall[:, 2048:3072], Tok("pd0")), (pall[:, 3072:4096], Tok("pd1"))])
        PS = Ring(PSI)
        PS_O = Ring(PSI[0:4])
        PS_S = Ring(PSI[4:7])
        PS_T = Ring(PSI[7:8])

        identf = gsb("identf", [128, 128], F32)
        identb = gsb("identb", [128, 128], BF16)
        bd = gsb("bd", [128, 128], BF16)
        modcol = gsb("modcol", [128, DEPTH, 48, 2], F32)
        A1col = gsb("A1col", [128, DEPTH, 8, 2], F32)
        A2col = gsb("A2col", [128, DEPTH, 8, 2], F32)
        epsc = gsb("epsc", [128, 1], F32)
        tconst = Tok("const")
        tmodl = [Tok("mod0"), Tok("mod1")]
        S.dma(identf[:], identf_in, newd(), writes=[tconst])
        S.dma(identb[:], identb_in, newd(), writes=[tconst])
        S.dma(bd[:], bd_in, newd(), writes=[tconst])
        S.dve("memset", epsc[:], EPS, writes=[tconst])
        S.barrier()

        def sbring(pes, name, n, shape, dt, with_dsem=False):
            items = []
            for i in range(n):
                t = pes.enter_context(nc.sbuf_tensor(uniq(f"{name}{i}"), list(shape), dt))
                if with_dsem:
                    items.append((t, Tok(f"{name}{i}"), newd()))
                else:
                    items.append((t, Tok(f"{name}{i}")))
            return Ring(items)

        def rowform(dst, tdst, col8, tcol, tmpring):
            for half in range(2):
                pb, tpb = PS.next()
                for b in range(4):
                    kc = half * 4 + b
                    cb, tcb = tmpring.next()
                    S.dve("tensor_copy", cb[:], col8[:, kc:kc + 1].to_broadcast([128, 128]), reads=[tcol], writes=[tcb])
                    S.mm(pb[:, b * 128:(b + 1) * 128], cb[:], identf[:], reads=[tcb, tconst], writes=[tpb])
                S.dve("tensor_copy", dst[:, half * 512:(half + 1) * 512], pb[:], reads=[tpb], writes=[tdst])

        cc = gsb("cc", [128, 8, 2], F32)
        sc = gsb("sc", [128, 8, 2], F32)
        bcol = gsb("bcol", [128, DEPTH, 48], F32)
        n1col = gsb("n1col", [128, DEPTH, 8], F32)
        n2col = gsb("n2col", [128, DEPTH, 8], F32)
        tsc, tb = Tok(), Tok()

        def mod_layer(l, pes, evac):
            WA = sbring(pes, "wa", 3, [128, 8, 512], F32R, with_dsem=True)
            for n in range(12):
                wt, twt, dwt = WA.next()
                S.dma(wt[:], w_ada[l][:, n * 512:(n + 1) * 512].rearrange("(kc p) j -> p kc j", p=128), dwt,
                      writes=[twt])
                for mi in range(4):
                    m = n * 4 + mi
                    pb, tpb = PS.next()
                    for kc in range(8):
                        S.mm(pb[:, 0:2], wt[:, kc, mi * 128:(mi + 1) * 128].bitcast(F32), sc[:, kc, :],
                             start=(kc == 0), stop=(kc == 7), reads=[twt, tsc], writes=[tpb])
                    if evac == "act":
                        S.act(modcol[:, l, m, :], pb[:, 0:2], AF.Identity, bias=bcol[:, l, m:m + 1], scale=1.0,
                              reads=[tpb, tb], writes=[tmodl[l]])
                    else:
                        S.dve("tensor_scalar", modcol[:, l, m, :], pb[:, 0:2], bcol[:, l, m:m + 1], 0.0,
                              ALU.add, ALU.add, reads=[tpb, tb], writes=[tmodl[l]])

        def mod_derive(l):
            for s in range(2):
                S.dve("scalar_tensor_tensor", A1col[:, l, :, s], modcol[:, l, 8:16, s], 1.0, n1col[:, l, :],
                      ALU.add, ALU.mult, reads=[tmodl[l], tb], writes=[tmodl[l]])
                S.dve("scalar_tensor_tensor", A2col[:, l, :, s], modcol[:, l, 32:40, s], 1.0, n2col[:, l, :],
                      ALU.add, ALU.mult, reads=[tmodl[l], tb], writes=[tmodl[l]])

        def phase_mod():
            with ExitStack() as pes:
                tcc = Tok()
                S.dma(cc[:], ccin, newd(), writes=[tcc])
                S.dma(bcol[:], bcol_in, newd(), writes=[tb])
                S.dma(n1col[:], n1col_in, newd(), writes=[tb])
                S.dma(n2col[:], n2col_in, newd(), writes=[tb])
                S.act(sc[:], cc[:], AF.Silu, reads=[tcc], writes=[tsc])
                mod_layer(0, pes, "dve")
                mod_derive(0)
                S.barrier()

        def phase_inproj(l):
            need_ctx = l < depth - 1
            with ExitStack() as pes:
                def sb(name, shape, dt):
                    return pes.enter_context(nc.sbuf_tensor(uniq(name), list(shape), dt))
                hT = sb("hT", [128, 8, TA], F32R)
                thT = [Tok(f"hT{j}") for j in range(NJ)]
                ss = sb("ss", [128, NJ], F32)
                rstd = sb("rstd", [128, NJ], F32)
                cosT = sb("cosT", [128, T], F32)
                sinT = sb("sinT", [128, T], F32)
                qkg = sb("qkg", [128, DEPTH, 4], F32)
                wv = sb("wv", [128, 8, 256], F32R)
                XT = sbring(pes, "xt", 3, [128, D], F32, with_dsem=True)
                JK = sbring(pes, "jk", 2, [128, D], F32)
                XN = sbring(pes, "xn", 2, [128, D], F32)
                WR = sbring(pes, "wr", 6, [128, 8, 128], F32R, with_dsem=True)
                OS = sbring(pes, "os", 4, [128, 512], BF16, with_dsem=True)
                SQ = sbring(pes, "sq", 3, [128, 512], BF16)
                RS = sbring(pes, "rs", 2, [128, 512], F32)
                T1 = sbring(pes, "t1", 2, [128, 512], F32)
                T2 = sbring(pes, "t2", 2, [128, 512], F32)
                tl = Tok("loads")
                tss = Tok("ss")
                S.dma(cosT[:], cos_in, newd(), writes=[tl])
                S.dma(sinT[:], sin_in, newd(), writes=[tl])
                S.dma(qkg[:], qkg_in, newd(), writes=[tl])
                S.dma(wv[:], w_v[l].rearrange("(kc p) j -> p kc j", p=128), newd(), writes=[tl])
                S.dve("memset", ss[:], 0.0, writes=[tss])
                eps64 = sb("eps64", [128, 1], F32)
                S.dve("memset", eps64[:], 64 * EPS, writes=[tl])
                for j in range(NJ):
                    s = 1 if j < 2 else 0
                    xt, txt, dxt = XT.next()
                    S.dma(xt[:], (xin if l == 0 else xs)[j * 128:(j + 1) * 128, :], dxt, writes=[txt])
                    jk, tjk = JK.next()
                    tsj = Tok()
                    S.act(jk[:], xt[:], AF.Square, accum_out=ss[:, j:j + 1], reads=[txt, tss], writes=[tjk, tsj])
                    S.act(rstd[:, j:j + 1], ss[:, j:j + 1], AF.Ln, bias=epsc[:, 0:1], scale=1.0 / D,
                          reads=[tsj, tconst], writes=[tsj])
                    S.act(rstd[:, j:j + 1], rstd[:, j:j + 1], AF.Exp, scale=-0.5, reads=[tsj], writes=[tsj])
                    xn, txn = XN.next()
                    S.act(xn[:], xt[:], AF.Copy, scale=rstd[:, j:j + 1], reads=[txt, tsj], writes=[txn])
                    for half in range(2):
                        pb, tpb = PS.next()
                        for b in range(4):
                            kc = half * 4 + b
                            S.tr(pb[:, b * 128:(b + 1) * 128], xn[:, kc * 128:(kc + 1) * 128], identf[:],
                                 reads=[txn, tconst], writes=[tpb])
                        for b in range(4):
                            kc = half * 4 + b
                            S.dve("tensor_scalar", hT[:, kc, j * 128:(j + 1) * 128], pb[:, b * 128:(b + 1) * 128],
                                  A1col[:, l, kc, s:s + 1], modcol[:, l, kc, s:s + 1], ALU.mult, ALU.add,
                                  reads=[tpb, tmodl[l]], writes=[thT[j]])

                def gtoks(goff, gn):
                    return thT[goff // 128:(goff + gn) // 128]

                worder = list(CH_F) + [CH_GATE0 + gc for gc in range(24)]
                for ci in range(4):
                    worder += [CH_GQ[ci], CH_GQS[ci]]
                worder += [CH_GK, CH_GKS]
                for ci in range(2):
                    worder += [CH_WQ[ci], CH_WQS[ci]]
                worder += [CH_WK, CH_WKS]
                wq = []
                wstate = [0, 0]

                def load_w(c):
                    while wstate[0] < min(len(worder), wstate[1] + 5):
                        cn = worder[wstate[0]]
                        wt, twt, dwt = WR.next()
                        S.dma(wt[:], w_inp[l][:, cn * 128:(cn + 1) * 128].rearrange("(kc p) j -> p kc j", p=128), dwt,
                              writes=[twt])
                        wq.append((cn, wt, twt))
                        wstate[0] += 1
                    cn, wt, twt = wq.pop(0)
                    assert cn == c, (cn, c)
                    wstate[1] += 1
                    return wt, twt

                def proj(wt, twt, goff, gn):
                    pb, tpb = PS.next()
                    for kc in range(8):
                        S.mm(pb[:, :gn], wt[:, kc, :], hT[:, kc, goff:goff + gn], start=(kc == 0), stop=(kc == 7),
                             reads=[twt] + gtoks(goff, gn), writes=[tpb])
                    return pb, tpb

                def groups_for(ctx_needed):
                    return [g for gi, g in enumerate(GROUPS) if gi > 0 or ctx_needed]

                for ci, c in enumerate(CH_F):
                    wt, twt = load_w(c)
                    for goff, gn in groups_for(need_ctx):
                        pb, tpb = proj(wt, twt, goff, gn)
                        st, tst, dst_ = OS.next()
                        S.dve("tensor_copy", st[:, :gn], pb[:, :gn], reads=[tpb], writes=[tst])
                        S.dma(GTs[ci][:, goff:goff + gn], st[:, :gn], dst_, reads=[tst])
                for gc in range(24):
                    wt, twt = load_w(CH_GATE0 + gc)
                    for goff, gn in groups_for(need_ctx):
                        pb, tpb = proj(wt, twt, goff, gn)
                        st, tst, dst_ = OS.next()
                        S.act(st[:, :gn], pb[:, :gn], AF.Sigmoid, reads=[tpb], writes=[tst])
                        S.dma(sgTs[gc][:, goff:goff + gn], st[:, :gn], dst_, reads=[tst])

                def qk_chunk(c, cs, gi, dst_fn, norm, ctx_needed):
                    wt, twt = load_w(c)
                    ws, tws = load_w(cs)
                    def stage1(goff, gn):
                        latent = goff >= 256
                        pz, tpz = proj(wt, twt, goff, gn)
                        pzs, tpzs = proj(ws, tws, goff, gn) if latent else (None, None)
                        sq, tsq = (None, None)
                        if norm:
                            sq, tsq = SQ.next()
                            S.act(sq[:, :gn], pz[:, :gn], AF.Square, reads=[tpz], writes=[tsq])
                        return (goff, gn, latent, pz, tpz, pzs, tpzs, sq, tsq)

                    def stage2(goff, gn, latent, pz, tpz, pzs, tpzs, sq, tsq):
                        lo = goff - 256
                        st, tst, dst_ = OS.next()
                        if norm:
                            pss, tpss = PS.next()
                            S.mm(pss[:, :gn], bd[:], sq[:, :gn], reads=[tsq, tconst], writes=[tpss])
                            rs, trs = RS.next()
                            S.act(rs[:, :gn], pss[:, :gn], AF.Ln, bias=eps64[:, 0:1], scale=1.0, reads=[tpss, tl],
                                  writes=[trs])
                            S.act(rs[:, :gn], rs[:, :gn], AF.Exp, scale=-0.5, reads=[trs], writes=[trs])
                            t1, tt1 = T1.next()
                            S.dve("scalar_tensor_tensor", t1[:, :gn], pz[:, :gn], qkg[:, l, gi:gi + 1], rs[:, :gn],
                                  ALU.mult, ALU.mult, reads=[tpz, trs, tl], writes=[tt1])
                            if latent:
                                t2, tt2 = T2.next()
                                S.dve("scalar_tensor_tensor", t2[:, :gn], pzs[:, :gn], qkg[:, l, gi + 1:gi + 2],
                                      rs[:, :gn], ALU.mult, ALU.mult, reads=[tpzs, trs, tl], writes=[tt2])
                                S.pool("tensor_tensor", t1[:, :gn], t1[:, :gn], cosT[:, lo:lo + gn], ALU.mult,
                                       reads=[tt1, tl], writes=[tt1])
                                S.pool("tensor_tensor", t2[:, :gn], t2[:, :gn], sinT[:, lo:lo + gn], ALU.mult,
                                       reads=[tt2, tl], writes=[tt2])
                                S.pool("tensor_tensor", st[:, :gn], t1[:, :gn], t2[:, :gn], ALU.add,
                                       reads=[tt1, tt2], writes=[tst])
                            else:
                                S.dve("tensor_copy", st[:, :gn], t1[:, :gn], reads=[tt1], writes=[tst])
                        else:
                            if latent:
                                t1, tt1 = T1.next()
                                t2, tt2 = T2.next()
                                S.dve("tensor_tensor", t1[:, :gn], pz[:, :gn], cosT[:, lo:lo + gn], ALU.mult,
                                      reads=[tpz, tl], writes=[tt1])
                                S.dve("tensor_tensor", t2[:, :gn], pzs[:, :gn], sinT[:, lo:lo + gn], ALU.mult,
                                      reads=[tpzs, tl], writes=[tt2])
                                S.pool("tensor_tensor", st[:, :gn], t1[:, :gn], t2[:, :gn], ALU.add,
                                       reads=[tt1, tt2], writes=[tst])
                            else:
                                S.dve("tensor_copy", st[:, :gn], pz[:, :gn], reads=[tpz], writes=[tst])
                        S.dma(dst_fn(goff, gn), dst_fn(None, gn, st), dst_, reads=[tst])

                    prev = None
                    for goff, gn in groups_for(ctx_needed):
                        cur = stage1(goff, gn)
                        if prev is not None:
                            stage2(*prev)
                        prev = cur
                    stage2(*prev)

                def qdst(scr, c):
                    def f(goff, gn, st=None):
                        if st is not None:
                            return st[:, :gn].rearrange("p (j q) -> p j q", q=128)
                        return scr[:, goff // 128:(goff + gn) // 128, c, :]
                    return f

                def kdst(scr):
                    def f(goff, gn, st=None):
                        if st is not None:
                            return st[:, :gn]
                        return scr[:, goff:goff + gn]
                    return f
                for ci in range(4):
                    qk_chunk(CH_GQ[ci], CH_GQS[ci], 0, qdst(QgTs, ci), True, need_ctx)
                qk_chunk(CH_GK, CH_GKS, 2, kdst(KgTs), True, True)
                for ci in range(2):
                    qk_chunk(CH_WQ[ci], CH_WQS[ci], None, qdst(QwTs, ci), False, need_ctx)
                qk_chunk(CH_WK, CH_WKS, None, kdst(KwTs), False, True)
                for j in range(NJ):
                    pb, tpb = PS.next()
                    for kc in range(8):
                        S.mm(pb[:, :256], hT[:, kc, j * 128:(j + 1) * 128], wv[:, kc, :], start=(kc == 0),
                             stop=(kc == 7), reads=[thT[j], tl], writes=[tpb])
                    st, tst, dst_ = OS.next()
                    S.dve("tensor_copy", st[:, :256], pb[:, :256], reads=[tpb], writes=[tst])
                    S.dma(Vs[j * 128:(j + 1) * 128, :], st[:, :256], dst_, reads=[tst])
                S.barrier()

        def phase_fourier(l):
            need_ctx = l < depth - 1
            with ExitStack() as pes:
                def sb(name, shape, dt):
                    return pes.enter_context(nc.sbuf_tensor(uniq(name), list(shape), dt))
                GT = sb("GT", [128, 2, TA], BF16)
                csc = sb("csc", [128, 256], BF16)
                AB = sb("AB", [128, NJ, 2, 256], BF16)
                ct256 = sb("ct256", [128, 2, 2, 256], BF16)
                CT = sbring(pes, "ct", 4, [128, 2, 16, 512], BF16, with_dsem=True)
                OS = sbring(pes, "osf", 3, [128, 512], BF16, with_dsem=True)
                tl = Tok()
                tAB = [Tok() for _ in range(NJ)]
                j0 = 0 if need_ctx else 2
                for c in range(2):
                    S.dma(GT[:, c, j0 * 128:], GTs[c][:, j0 * 128:], newd(), writes=[tl])
                S.dma(csc[:], csc_in, newd(), writes=[tl])
                if need_ctx:
                    for cs in range(2):
                        S.dma(ct256[:, cs], cs256[cs].rearrange("(j p) k -> p j k", p=128), newd(), writes=[tl])
                for j in range(j0, NJ):
                    for c in range(2):
                        pb, tpb = PS.next()
                        S.mm(pb[:, :256], GT[:, c, j * 128:(j + 1) * 128], csc[:], reads=[tl], writes=[tpb])
                        if c == 0:
                            S.dve("tensor_copy", AB[:, j, c, :], pb[:, :256], reads=[tpb], writes=[tAB[j]])
                        else:
                            S.act(AB[:, j, c, :], pb[:, :256], AF.Copy, reads=[tpb], writes=[tAB[j]])
                cts = []
                for kg in range(4):
                    ct, tct, dct = CT.next()
                    for cs in range(2):
                        S.dma(ct[:, cs], cs2048[cs][:, kg * 512:(kg + 1) * 512].rearrange("(j p) k -> p j k", p=128),
                              dct, writes=[tct])
                    cts.append((ct, tct))
                for kg in range(4):
                    ct, tct = cts[kg]
                    for c in range(2):
                        pb, tpb = PS.next()
                        for j in range(16):
                            for cs in range(2):
                                S.mm(pb[:], AB[:, 2 + j, c, cs * 128:(cs + 1) * 128], ct[:, cs, j, :],
                                     start=(j == 0 and cs == 0), stop=(j == 15 and cs == 1),
                                     reads=[tAB[2 + j], tct], writes=[tpb])
                        st, tst, dst_ = OS.next()
                        S.dve("tensor_copy", st[:], pb[:], reads=[tpb], writes=[tst])
                        S.dma(FTs[c][:, 256 + kg * 512:256 + (kg + 1) * 512], st[:], dst_, reads=[tst])
                if need_ctx:
                    for c in range(2):
                        pb, tpb = PS.next()
                        for j in range(2):
                            for cs in range(2):
                                S.mm(pb[:, :256], AB[:, j, c, cs * 128:(cs + 1) * 128], ct256[:, cs, j, :],
                                     start=(j == 0 and cs == 0), stop=(j == 1 and cs == 1),
                                     reads=[tAB[j], tl], writes=[tpb])
                        st, tst, dst_ = OS.next()
                        S.dve("tensor_copy", st[:, :256], pb[:, :256], reads=[tpb], writes=[tst])
                        S.dma(FTs[c][:, 0:256], st[:, :256], dst_, reads=[tst])
                S.barrier()

        def phase_attn(l):
            need_ctx = l < depth - 1
            with ExitStack() as pes:
                def sb(name, shape, dt):
                    return pes.enter_context(nc.sbuf_tensor(uniq(name), list(shape), dt))
                Kg = sb("Kg", [128, TA], BF16)
                Kw = sb("Kw", [128, TA], BF16)
                Qg = [sb(f"Qg{g}", [128, NJ, 4, 128], BF16) for g in range(2)]
                Qw = [sb(f"Qw{g}", [128, NJ, 2, 128], BF16) for g in range(2)]
                Va = sb("Va", [128, NJ, 4, 65], BF16)
                OgT = sb("OgT", [128, 4, TA], BF16)
                OwT = sb("OwT", [128, 2, TA], BF16)
                mk = sb("mk", [128, 2, 256], BF16)
                esk = sb("esk", [128, 4], F32)
                EX = sbring(pes, "ex", 3, [128, 1024], BF16)
                OTM = sbring(pes, "otm", 2, [128, 512], BF16)
                RC = sbring(pes, "rc", 4, [128, 4], F32)
                tl, tV, tsk = Tok(), Tok(), Tok()
                tVd = Tok()
                tO = Tok()
                j0 = 0 if need_ctx else 2
                S.dma(Kg[:], KgTs, newd(), writes=[tl])
                S.dma(Kw[:], KwTs, newd(), writes=[tl])
                tq0 = Tok()
                for g in range(2):
                    o = 1 - g
                    S.pool("memset", Qg[g][o * 64:(o + 1) * 64], 0.0, writes=[tq0])
                    S.pool("memset", Qw[g][o * 64:(o + 1) * 64], 0.0, writes=[tq0])
                    S.dma(Qg[g][g * 64:(g + 1) * 64, j0:], QgTs[g * 64:(g + 1) * 64, j0:], newd(), writes=[tl])
                    S.dma(Qw[g][g * 64:(g + 1) * 64, j0:], QwTs[g * 64:(g + 1) * 64, j0:], newd(), writes=[tl])
                S.dma(mk[:], mk_in, newd(), writes=[tl])
                S.dma(esk[:], sink_in[l].partition_broadcast(128), newd(), writes=[tsk])
                S.act(esk[:], esk[:], AF.Exp, reads=[tsk], writes=[tsk])
                S.pool("memset", Va[:], 1.0, writes=[tV])
                dva = newd()
                for g4 in range(4):
                    S.dma(Va[:, :, g4, 0:64], Vs[:, g4 * 64:(g4 + 1) * 64].rearrange("(j p) d -> p j d", p=128), dva,
                          reads=[tV], writes=[tVd])
                tV = tVd

                fin = []

                def attend(jq, keys, Q, K, nh, voff, scale, masks, sink, OT):
                    nq = nh * 128
                    otm, totm = OTM.next()
                    pos = [PS_O.next() for _ in range(2)]

                    def issue_s(ki):
                        kc = keys[ki]
                        dt_, tdt = PS_D.next()
                        for g in range(2):
                            S.mm(dt_[:, g * nq:(g + 1) * nq], K[:, kc * 128:(kc + 1) * 128],
                                 Q[g][:, jq, :, :].rearrange("p c q -> p (c q)"),
                                 reads=[tl, tq0], writes=[tdt])
                        return dt_, tdt
                    pend = issue_s(0)
                    for ki, kc in enumerate(keys):
                        dt_, tdt = pend
                        if ki + 1 < len(keys):
                            pend = issue_s(ki + 1)
                        if ki == 0:
                            while fin:
                                fin.pop(0)()
                        ex, tex = EX.next()
                        S.act(ex[:, :2 * nq], dt_[:, :2 * nq], AF.Exp, scale=scale, reads=[tdt], writes=[tex])
                        if kc in masks:
                            for g in range(2):
                                S.pool("tensor_tensor", ex[:, g * nq:(g + 1) * nq], ex[:, g * nq:(g + 1) * nq],
                                       mk[:, masks[kc], :], ALU.mult, reads=[tex, tl], writes=[tex])
                        for g in range(2):
                            po, tpo = pos[g]
                            for h in range(nh):
                                S.mm(po[:, h * 65:(h + 1) * 65], ex[:, g * nq + h * 128:g * nq + (h + 1) * 128],
                                     Va[:, kc, voff + g, :], start=(ki == 0 and h == 0), stop=(ki == len(keys) - 1),
                                     reads=[tex, tV], writes=[tpo], skip_group_check=True)
                    for g in range(2):
                        po, tpo = pos[g]
                        rc, trc = RC.next()
                        po3 = po[:, 0:nh * 65].rearrange("p (h d) -> p h d", d=65)
                        if sink:
                            S.dve("tensor_tensor", rc[:, 0:nh], po3[:, :, 64], esk[:, g * 2:g * 2 + nh], ALU.add,
                                  reads=[tpo, tsk], writes=[trc])
                            S.dve("reciprocal", rc[:, 0:nh], rc[:, 0:nh], reads=[trc], writes=[trc])
                        else:
                            S.dve("reciprocal", rc[:, 0:nh], po3[:, :, 64], reads=[tpo], writes=[trc])
                        for h in range(nh):
                            S.dve("tensor_scalar", otm[:, (g * nh + h) * 64:(g * nh + h + 1) * 64],
                                  po[:, h * 65:h * 65 + 64], rc[:, h:h + 1], 0.0, ALU.mult, ALU.add,
                                  reads=[tpo, trc], writes=[totm])
                    nblk = 2 * nh * 64 // 128

                    def finish():
                        pt, tpt = pos[0]
                        ptb = pt.bitcast(BF16)
                        for c in range(nblk):
                            S.tr(ptb[:, c * 128:(c + 1) * 128], otm[:, c * 128:(c + 1) * 128], identb[:],
                                 reads=[totm, tconst], writes=[tpt])
                        S.dve("tensor_copy", OT[:, :, jq * 128:(jq + 1) * 128],
                              ptb[:, 0:nblk * 128].rearrange("p (c q) -> p c q", c=nblk),
                              reads=[tpt], writes=[tO])
                    fin.append(finish)

                for jq in range(j0, NJ):
                    if jq < 2:
                        gkeys = [0, 1]
                        wkeys = [0, 1]
                        masks = {}
                    else:
                        gkeys = list(range(NJ))
                        wkeys = [0, 1]
                        masks = {}
                        if jq - 1 >= 2:
                            wkeys.append(jq - 1)
                            masks[jq - 1] = 0
                        wkeys.append(jq)
                        if jq + 1 < NJ:
                            wkeys.append(jq + 1)
                            masks[jq + 1] = 1
                    attend(jq, gkeys, Qg, Kg, 4, 0, 8.0, {}, False, OgT)
                    attend(jq, wkeys, Qw, Kw, 2, 2, 0.125, masks, True, OwT)
                while fin:
                    fin.pop(0)()
                for c in range(4):
                    S.dma(OgTs[c][:, j0 * 128:], OgT[:, c, j0 * 128:], newd(), reads=[tO])
                for c in range(2):
                    S.dma(OwTs[c][:, j0 * 128:], OwT[:, c, j0 * 128:], newd(), reads=[tO])
                S.barrier()

        def phase_merge(l):
            need_ctx = l < depth - 1
            with ExitStack() as pes:
                def sb(name, shape, dt):
                    return pes.enter_context(nc.sbuf_tensor(uniq(name), list(shape), dt))
                wbr = sb("wbr", [128, 8, D], BF16)
                wo = sb("wo", [128, 8, D], BF16)
                g1row = [sb(f"g1row{s}", [128, D], F32) for s in range(2)]
                tg1 = Tok()
                WST = sbring(pes, "wst", 2, [128, 8, 256], F32, with_dsem=True)
                CB = sbring(pes, "cb", 2, [128, 128], F32)
                tw = Tok()
                for (wsrc, wdst) in ((w_br, wbr), (w_out, wo)):
                    for qq in range(4):
                        st, tst, dst_ = WST.next()
                        S.dma(st[:], wsrc[l][:, qq * 256:(qq + 1) * 256].rearrange("(kc p) j -> p kc j", p=128),
                              dst_, writes=[tst])
                        S.dve("tensor_copy", wdst[:, :, qq * 256:(qq + 1) * 256], st[:], reads=[tst], writes=[tw])
                for s in range(2 if need_ctx else 1):
                    rowform(g1row[s], tg1, modcol[:, l, 16:24, s], tmodl[l], CB)
                fT = sbring(pes, "fT", 2, [128, 8, 512], BF16, with_dsem=True)
                sg = sbring(pes, "sg", 2, [128, 24, 512], BF16, with_dsem=True)
                mT = sbring(pes, "mT", 2, [128, 8, 512], BF16)
                TT = sbring(pes, "tt", 4, [128, 512], F32)
                XT = sbring(pes, "xt4", 8, [128, D], F32, with_dsem=True)
                XO = sbring(pes, "xo4", 3, [128, D], F32, with_dsem=True)
                act_groups = [(gi, g) for gi, g in enumerate(GROUPS) if gi > 0 or need_ctx]
                loaded = {}

                def issue_loads(ai):
                    gi, (goff, gn) = act_groups[ai]
                    f_, tf, df = fT.next()
                    for c in range(2):
                        S.dma(f_[:, c, :gn], FTs[c][:, goff:goff + gn], df, writes=[tf])
                    for c in range(4):
                        S.dma(f_[:, 2 + c, :gn], OgTs[c][:, goff:goff + gn], df, writes=[tf])
                    for c in range(2):
                        S.dma(f_[:, 6 + c, :gn], OwTs[c][:, goff:goff + gn], df, writes=[tf])
                    sg_, tsg, dsg = sg.next()
                    S.dma(sg_[:, :, :gn], sgTs[:, :, goff:goff + gn].rearrange("c p t -> p c t"), dsg, writes=[tsg])
                    xl = []
                    for tcn in range(gn // 128):
                        j = goff // 128 + tcn
                        xt, txt, dxt = XT.next()
                        S.dma(xt[:], (xin if l == 0 else xs)[j * 128:(j + 1) * 128, :], dxt, writes=[txt])
                        xl.append((xt, txt))
                    loaded[ai] = (f_, tf, sg_, tsg, xl)
                issue_loads(0)
                for ai, (gi, (goff, gn)) in enumerate(act_groups):
                    s = 1 if gi == 0 else 0
                    if ai + 1 < len(act_groups):
                        issue_loads(ai + 1)
                    f_, tf, sg_, tsg, xl = loaded.pop(ai)
                    m_, tm_ = mT.next()
                    for d in range(8):
                        tts = []
                        for b, kcs in enumerate(([0, 1], [2, 3, 4, 5], [6, 7])):
                            pb, tpb = PS.next()
                            for ki, kc in enumerate(kcs):
                                S.mm(pb[:, :gn], wbr[:, kc, d * 128:(d + 1) * 128], f_[:, kc, :gn],
                                     start=(ki == 0), stop=(ki == len(kcs) - 1), reads=[tw, tf], writes=[tpb])
                            tt, ttt = TT.next()
                            S.dve("tensor_tensor", tt[:, :gn], pb[:, :gn], sg_[:, b * 8 + d, :gn], ALU.mult,
                                  reads=[tpb, tsg], writes=[ttt])
                            tts.append((tt, ttt))
                        S.pool("tensor_tensor", tts[0][0][:, :gn], tts[0][0][:, :gn], tts[1][0][:, :gn], ALU.add,
                               reads=[tts[0][1], tts[1][1]], writes=[tts[0][1]])
                        S.dve("tensor_tensor", m_[:, d, :gn], tts[0][0][:, :gn], tts[2][0][:, :gn], ALU.add,
                              reads=[tts[0][1], tts[2][1]], writes=[tm_])
                    for tcn in range(gn // 128):
                        j = goff // 128 + tcn
                        xt, txt = xl[tcn]
                        xo, txo, dxo = XO.next()
                        for half in range(2):
                            pb, tpb = PS.next()
                            for kc in range(8):
                                S.mm(pb[:], m_[:, kc, tcn * 128:(tcn + 1) * 128], wo[:, kc, half * 512:(half + 1) * 512],
                                     start=(kc == 0), stop=(kc == 7), reads=[tm_, tw], writes=[tpb])
                            tt, ttt = TT.next()
                            S.dve("tensor_tensor", tt[:], pb[:], g1row[s][:, half * 512:(half + 1) * 512], ALU.mult,
                                  reads=[tpb, tg1], writes=[ttt])
                            S.dve("tensor_tensor", xo[:, half * 512:(half + 1) * 512], tt[:],
                                  xt[:, half * 512:(half + 1) * 512], ALU.add, reads=[ttt, txt], writes=[txo])
                        S.dma(xs[j * 128:(j + 1) * 128, :], xo[:], dxo, reads=[txo])
                S.barrier()

        def phase_moe(l, sets, dbgout=False):
            ns = len(sets)
            CT = sum(st_[2] for st_ in sets)
            offs = [sum(st_[2] for st_ in sets[:i]) for i in range(ns)]
            with ExitStack() as pes:
                def sb(name, shape, dt):
                    return pes.enter_context(nc.sbuf_tensor(uniq(name), list(shape), dt))
                iota = sb("iota", [128, 256], F32)
                pidx = sb("pidx", [128, 2], F32)
                selb = sb("selb", [16, 16, 128], BF16)
                tl = Tok()
                S.dma(iota[:], iota_in, newd(), writes=[tl])
                S.dma(pidx[:], pidx_in, newd(), writes=[tl])
                S.dma(selb[:], selb_in, newd(), writes=[tl])
                P_ = []
                for si, (j0, nch, cap, s) in enumerate(sets):
                    N = nch * 128
                    d_ = dict(j0=j0, nch=nch, cap=cap, s=s, N=N, pc=min(cap, 128), ncc=max(1, cap // 128),
                              tgs=[(o, min(512, N - o)) for o in range(0, N, 512)], off=offs[si])
                    d_["h2"] = sb(f"h2_{si}", [128, nch, D], BF16)
                    d_["th2"] = [Tok() for _ in range(nch)]
                    d_["slotT"] = sb(f"slotT{si}", [16, N], BF16)
                    d_["wT"] = sb(f"wT{si}", [16, N], BF16)
                    d_["slot_tm"] = sb(f"slot_tm{si}", [128, nch, 16], F32)
                    d_["mask_tm"] = sb(f"mask_tm{si}", [128, nch, 16], F32)
                    d_["g2row"] = sb(f"g2row{si}", [128, D], F32)
                    d_["trt"] = Tok()
                    d_["tg2"] = Tok()
                    d_["tx"] = [Tok() for _ in range(nch)]
                    P_.append(d_)
                for si, d_ in enumerate(P_):
                    j0, nch, cap, s, N = d_["j0"], d_["nch"], d_["cap"], d_["s"], d_["N"]
                    h2, th2, trt = d_["h2"], d_["th2"], d_["trt"]
                    with ExitStack() as res:
                        def rsb(name, shape, dt):
                            return res.enter_context(nc.sbuf_tensor(uniq(name), list(shape), dt))
                        A2row = rsb("A2row", [128, D], F32)
                        sh2row = rsb("sh2row", [128, D], F32)
                        wr = rsb("wr", [128, 8, E], F32)
                        aff_tm = rsb("aff_tm", [128, nch, 16], F32)
                        affT = rsb("affT", [16, N], F32)
                        work = rsb("work", [16, N], F32)
                        maskT = rsb("maskT", [16, N], F32)
                        zer = rsb("zer", [16, N], F32)
                        ss = rsb("ss5", [128, nch], F32)
                        rstd = rsb("rstd5", [128, nch], F32)
                        mx8 = rsb("mx8", [16, 8], F32)
                        CB = sbring(res, "cb5", 2, [128, 128], F32)
                        XT = sbring(res, "xt5", 4, [128, D], F32, with_dsem=True)
                        JK = sbring(res, "jk5", 2, [128, D], F32)
                        HF = sbring(res, "hf5", 3, [128, D], F32)
                        HT = sbring(res, "ht5", 4, [128, 8, 128], F32)
                        SM = sbring(res, "sm5", 3, [128, 20], F32)
                        ET = sbring(res, "et5", 3, [128, 16], F32)
                        trow, tss, taff, tlr = Tok(), Tok(), Tok(), Tok()
                        S.dma(wr[:], w_r[l].rearrange("(kc p) e -> p kc e", p=128), newd(), writes=[tlr])
                        rowform(A2row, trow, A2col[:, l, :, s], tmodl[l], CB)
                        rowform(sh2row, trow, modcol[:, l, 24:32, s], tmodl[l], CB)
                        rowform(d_["g2row"], d_["tg2"], modcol[:, l, 40:48, s], tmodl[l], CB)
                        S.dve("memset", ss[:], 0.0, writes=[tss])
                        S.pool("memset", zer[:], 0.0, writes=[trt])
                        st_x, st_hf, st_pb, st_ht = {}, {}, {}, {}

                        def s_stats(jj):
                            j = j0 + jj
                            xt, txt, dxt = XT.next()
                            S.dma(xt[:], xs[j * 128:(j + 1) * 128, :], dxt, writes=[txt])
                            jk, tjk = JK.next()
                            tsj = Tok()
                            S.act(jk[:], xt[:], AF.Square, accum_out=ss[:, jj:jj + 1], reads=[txt, tss],
                                  writes=[tjk, tsj])
                            S.act(rstd[:, jj:jj + 1], ss[:, jj:jj + 1], AF.Ln, bias=epsc[:, 0:1], scale=1.0 / D,
                                  reads=[tsj, tconst], writes=[tsj])
                            S.act(rstd[:, jj:jj + 1], rstd[:, jj:jj + 1], AF.Exp, scale=-0.5, reads=[tsj],
                                  writes=[tsj])
                            st_x[jj] = (xt, txt, tsj)

                        def s_mod(jj):
                            xt, txt, tsj = st_x.pop(jj)
                            hf, thf = HF.next()
                            S.dve("scalar_tensor_tensor", hf[:], xt[:], rstd[:, jj:jj + 1], A2row[:], ALU.mult,
                                  ALU.mult, reads=[txt, tsj, trow], writes=[thf])
                            S.dve("tensor_tensor", hf[:], hf[:], sh2row[:], ALU.add, reads=[thf, trow], writes=[thf])
                            S.act(h2[:, jj, :], hf[:], AF.Copy, reads=[thf], writes=[th2[jj]])
                            pbs_ = []
                            for half in range(2):
                                pb, tpb = PS.next()
                                for b in range(4):
                                    kc = half * 4 + b
                                    S.tr(pb[:, b * 128:(b + 1) * 128], hf[:, kc * 128:(kc + 1) * 128], identf[:],
                                         reads=[thf, tconst], writes=[tpb])
                                pbs_.append((pb, tpb))
                            st_pb[jj] = pbs_

                        def s_evac(jj):
                            ht, tht = HT.next()
                            for half, (pb, tpb) in enumerate(st_pb.pop(jj)):
                                S.dve("tensor_copy", ht[:, half * 4:(half + 1) * 4, :],
                                      pb[:].rearrange("p (b q) -> p b q", b=4), reads=[tpb], writes=[tht])
                            st_ht[jj] = (ht, tht)

                        def s_rmm(jj):
                            ht, tht = st_ht.pop(jj)
                            pl, tpl = PS.next()
                            for kc in range(8):
                                S.mm(pl[:, 0:16], ht[:, kc, :], wr[:, kc, :], start=(kc == 0), stop=(kc == 7),
                                     reads=[tht, tlr], writes=[tpl])
                            return pl, tpl

                        def s_softmax(jj, pl, tpl):
                            sm, tsm = SM.next()
                            et, tet = ET.next()
                            S.dve("reduce_max", sm[:, 0:1], pl[:, 0:16], axis=AX.X, reads=[tpl], writes=[tsm])
                            S.dve("tensor_scalar", sm[:, 1:2], sm[:, 0:1], -1.0, 0.0, ALU.mult, ALU.add, reads=[tsm],
                                  writes=[tsm])
                            S.dve("memset", sm[:, 2:3], 0.0, writes=[tsm])
                            S.act(et[:], pl[:, 0:16], AF.Exp, bias=sm[:, 1:2], scale=1.0, accum_out=sm[:, 2:3],
                                  reads=[tpl, tsm], writes=[tet, tsm])
                            return sm, tsm, et, tet

                        def s_softmax2(jj, sm, tsm, et, tet):
                            S.dve("reciprocal", sm[:, 3:4], sm[:, 2:3], reads=[tsm], writes=[tsm])
                            S.dve("tensor_scalar", aff_tm[:, jj, :], et[:], sm[:, 3:4], 0.0, ALU.mult, ALU.add,
                                  reads=[tet, tsm], writes=[taff])

                        for i in range(-2, nch + 1):
                            if 0 <= i + 2 < nch:
                                s_stats(i + 2)
                            plt = s_rmm(i - 1) if 0 <= i - 1 < nch else None
                            if 0 <= i + 1 < nch:
                                s_mod(i + 1)
                            smt = s_softmax(i - 1, *plt) if plt is not None else None
                            if 0 <= i < nch:
                                s_evac(i)
                            if smt is not None:
                                s_softmax2(i - 1, *smt)
                        if si == 0 and l + 1 < depth:
                            mod_layer(l + 1, res, "act")
                        for tg0, tgn in d_["tgs"]:
                            pb, tpb = PS.next()
                            for q in range(tgn // 128):
                                jj = tg0 // 128 + q
                                S.tr(pb[0:16, q * 128:(q + 1) * 128], aff_tm[:, jj, :], identf[:],
                                     reads=[taff, tconst], writes=[tpb])
                            S.dve("tensor_copy", affT[:, tg0:tg0 + tgn], pb[0:16, :tgn], reads=[tpb], writes=[trt])
                        S.act(work[:], affT[:], AF.Copy, reads=[trt], writes=[trt])
                        for it in range(cap // 8):
                            S.dve("max", mx8[:], work[:], reads=[trt], writes=[trt])
                            S.dve("match_replace", work[:], mx8[:], work[:], 0.0, reads=[trt], writes=[trt])
                        S.dve("tensor_tensor", maskT[:], affT[:], work[:], ALU.subtract, reads=[trt], writes=[trt])
                        S.dve("tensor_scalar", maskT[:], maskT[:], 0.0, 0.0, ALU.is_gt, ALU.add, reads=[trt],
                              writes=[trt])
                        S.dve("tensor_tensor", d_["wT"][:], maskT[:], affT[:], ALU.mult, reads=[trt], writes=[trt])
                        S.dve("tensor_tensor_scan", work[:], maskT[:], zer[:], 0.0, ALU.add, ALU.add, reads=[trt],
                              writes=[trt])
                        S.dve("tensor_tensor", work[:], work[:], maskT[:], ALU.subtract, reads=[trt], writes=[trt])
                        S.dve("tensor_copy", d_["slotT"][:], work[:], reads=[trt], writes=[trt])
                        if dbgout and si == 0:
                            S.dma(dbg_aff[:, 0:N], affT[:], newd(), reads=[trt])
                            S.dma(dbg_mask[:, 0:N], maskT[:], newd(), reads=[trt])
                        for (src, dstt) in ((work, d_["slot_tm"]), (maskT, d_["mask_tm"])):
                            pb, tpb = PS.next()
                            for jj in range(nch):
                                S.tr(pb[:, jj * 16:(jj + 1) * 16], src[:, jj * 128:(jj + 1) * 128],
                                     identf[0:16, 0:16], reads=[trt, tconst], writes=[tpb])
                            S.dve("tensor_copy", dstt[:], pb[:, 0:nch * 16].rearrange("p (j e) -> p j e", e=16),
                                  reads=[tpb], writes=[trt])
                        if si == 0 and l + 1 < depth:
                            mod_derive(l + 1)
                        S.barrier()
                NPAT = 4 if ns > 1 else 5
                for d_ in P_:
                    d_["PP"] = sbring(pes, "pp", 1, [128, d_["nch"], d_["cap"]], BF16)
                    d_["PAT"] = [(sb(f"pat{i}", [128, d_["ncc"], d_["N"]], BF16), Tok()) for i in range(NPAT)]
                    d_["OE"] = [(sb(f"oe{i}", [128, d_["ncc"], D], BF16), Tok()) for i in range(4)]
                xg = sb("xg", [128, 8, CT], F32R)
                txg = Tok()
                actT = sb("actT", [128, 8, CT], F32R)
                tact = Tok()
                WE = sbring(pes, "we", 5 if ns > 1 else 6, [128, 8, 256], F32R, with_dsem=True)
                WB = sbring(pes, "wb", 2, [128, 512], BF16)
                SA = sbring(pes, "sa", 2, [128, CT], F32)
                fuse_final = (l == depth - 1)
                if fuse_final:
                    fgrow = sb("fgrow6", [128, D], F32)
                    jkf = sb("jkf6", [128, D], BF16)
                    ssf = sb("ssf6", [128, 16], F32)
                    rstdf = sb("rstdf6", [128, 16], F32)
                    tfg, tssf = Tok(), Tok()
                    S.dma(fgrow[:], fg_in.partition_broadcast(128), newd(), writes=[tfg])
                    S.dve("memset", ssf[:], 0.0, writes=[tssf])
                XT2 = sbring(pes, "xt6", 4, [128, D], F32, with_dsem=True)
                XPF = 3

                def load_we(wsrc, e, q):
                    wt, twt, dwt = WE.next()
                    S.dma(wt[:], wsrc[l][e][:, q * 256:(q + 1) * 256].rearrange("(kc p) j -> p kc j", p=128), dwt,
                          writes=[twt])
                    return wt, twt

                def build_pp(e):
                    for d_ in P_:
                        nch, cap, trt = d_["nch"], d_["cap"], d_["trt"]
                        pp, tpp = d_["PP"].next()
                        d_["pp"] = (pp, tpp)
                        for jj in range(nch):
                            S.ex("dve", "tensor_scalar", pp[:, jj, :], iota[:, 0:cap], d_["slot_tm"][:, jj, e:e + 1],
                                 d_["mask_tm"][:, jj, e:e + 1], ALU.is_equal, ALU.mult, reads=[trt, tl],
                                 writes=[tpp])

                def build_pat(e, el):
                    for d_ in P_:
                        trt = d_["trt"]
                        pat, tpat = d_["PAT"][e % NPAT]
                        for tg0, tgn in d_["tgs"]:
                            pbs, tpbs = PS.next()
                            S.mm(pbs[:, :tgn], selb[:, e, :], d_["slotT"][:, tg0:tg0 + tgn], reads=[trt, tl],
                                 writes=[tpbs])
                            pbw, tpbw = PS.next()
                            S.mm(pbw[:, :tgn], selb[:, e, :], d_["wT"][:, tg0:tg0 + tgn], reads=[trt, tl],
                                 writes=[tpbw])
                            wb, twb = WB.next()
                            S.act(wb[:, :tgn], pbw[:, :tgn], AF.Copy, reads=[tpbw], writes=[twb])
                            for cc in range(d_["ncc"]):
                                S.dve("scalar_tensor_tensor", pat[:, cc, tg0:tg0 + tgn], pbs[:, :tgn],
                                      pidx[:, cc:cc + 1], wb[:, :tgn], ALU.is_equal, ALU.mult,
                                      reads=[tpbs, twb, tl], writes=[tpat])

                build_pp(0)
                build_pat(0, 0)
                for grp in range(4):
                    for el in range(4):
                        e = grp * 4 + el
                        for d in range(8):
                            for d_ in P_:
                                nch, cap, off = d_["nch"], d_["cap"], d_["off"]
                                pp, tpp = d_["pp"]
                                pb, tpb = PS.next()
                                for jj in range(nch):
                                    S.mm(pb[:, :cap], d_["h2"][:, jj, d * 128:(d + 1) * 128], pp[:, jj, :],
                                         start=(jj == 0), stop=(jj == nch - 1), reads=[d_["th2"][jj], tpp],
                                         writes=[tpb])
                                if d % 2 == 0:
                                    S.dve("tensor_copy", xg[:, d, off:off + cap], pb[:, :cap], reads=[tpb],
                                          writes=[txg])
                                else:
                                    S.act(xg[:, d, off:off + cap], pb[:, :cap], AF.Copy, reads=[tpb], writes=[txg])
                        if e + 1 < E:
                            if el < 3 or NPAT == 5:
                                build_pat(e + 1, el + 1)
                            build_pp(e + 1)
                        for q in range(4):
                            wg_, twg = load_we(w_ge, e, q)
                            wu_, twu = load_we(w_ue, e, q)
                            for fl in range(2):
                                f = q * 2 + fl
                                pa, tpa = PS.next()
                                for kc in range(8):
                                    S.mm(pa[:, :CT], wg_[:, kc, fl * 128:(fl + 1) * 128], xg[:, kc, :],
                                         start=(kc == 0), stop=(kc == 7), reads=[twg, txg], writes=[tpa])
                                pu, tpu = PS.next()
                                for kc in range(8):
                                    S.mm(pu[:, :CT], wu_[:, kc, fl * 128:(fl + 1) * 128], xg[:, kc, :],
                                         start=(kc == 0), stop=(kc == 7), reads=[twu, txg], writes=[tpu])
                                sa, tsa = SA.next()
                                S.act(sa[:, :CT], pa[:, :CT], AF.Silu, reads=[tpa], writes=[tsa])
                                S.dve("tensor_tensor", actT[:, f, :], sa[:, :CT], pu[:, :CT], ALU.mult,
                                      reads=[tsa, tpu], writes=[tact])
                        for q in range(4):
                            wd_, twd = load_we(w_de, e, q)
                            ci = 0
                            for d_ in P_:
                                oe, toe = d_["OE"][el]
                                pc = d_["pc"]
                                for cc in range(d_["ncc"]):
                                    c0 = d_["off"] + cc * 128
                                    pb, tpb = PS.next()
                                    for f in range(8):
                                        S.mm(pb[:pc, :256], actT[:, f, c0:c0 + pc], wd_[:, f, :], start=(f == 0),
                                             stop=(f == 7), reads=[tact, twd], writes=[tpb])
                                    S.dve("tensor_tensor", oe[:pc, cc, q * 256:(q + 1) * 256], pb[:pc, :256],
                                          d_["g2row"][:pc, q * 256:(q + 1) * 256], ALU.mult,
                                          reads=[tpb, d_["tg2"]], writes=[toe])
                                    ci += 1
                    for d_ in P_:
                        pc, ncc = d_["pc"], d_["ncc"]
                        xq = []
                        nxt = [0]
                        for jj in range(d_["nch"]):
                            while nxt[0] < min(d_["nch"], jj + XPF):
                                jn = nxt[0]
                                xt_, txt_, dxt_ = XT2.next()
                                S.dma(xt_[:], xs[(d_["j0"] + jn) * 128:(d_["j0"] + jn + 1) * 128, :], dxt_,
                                      reads=[d_["tx"][jn]], writes=[txt_])
                                xq.append((xt_, txt_, dxt_))
                                nxt[0] += 1
                            j = d_["j0"] + jj
                            xt, txt, dxt = xq.pop(0)
                            for half in range(2):
                                pb, tpb = PS.next()
                                n_mm = 4 * ncc
                                i_mm = 0
                                for el in range(4):
                                    for cc in range(ncc):
                                        S.mm(pb[:], d_["PAT"][(grp * 4 + el) % NPAT][0][:pc, cc, jj * 128:(jj + 1) * 128],
                                             d_["OE"][el][0][:pc, cc, half * 512:(half + 1) * 512],
                                             start=(i_mm == 0), stop=(i_mm == n_mm - 1),
                                             reads=[d_["PAT"][(grp * 4 + el) % NPAT][1], d_["OE"][el][1]], writes=[tpb])
                                        i_mm += 1
                                S.dve("tensor_tensor", xt[:, half * 512:(half + 1) * 512], pb[:],
                                      xt[:, half * 512:(half + 1) * 512], ALU.add, reads=[tpb, txt], writes=[txt])
                            if fuse_final and grp == 3 and d_["s"] == 0:
                                tsj = Tok()
                                S.act(jkf[:], xt[:], AF.Square, accum_out=ssf[:, jj:jj + 1], reads=[txt, tssf],
                                      writes=[tsj])
                                S.act(rstdf[:, jj:jj + 1], ssf[:, jj:jj + 1], AF.Ln, bias=epsc[:, 0:1],
                                      scale=1.0 / D, reads=[tsj, tconst], writes=[tsj])
                                S.act(rstdf[:, jj:jj + 1], rstdf[:, jj:jj + 1], AF.Exp, scale=-0.5, reads=[tsj],
                                      writes=[tsj])
                                S.dve("scalar_tensor_tensor", xt[:], xt[:], rstdf[:, jj:jj + 1], fgrow[:], ALU.mult,
                                      ALU.mult, reads=[txt, tsj, tfg], writes=[txt])
                                S.dma(out[jj * 128:(jj + 1) * 128, :], xt[:], dxt, reads=[txt], writes=[d_["tx"][jj]])
                            else:
                                S.dma(xs[j * 128:(j + 1) * 128, :], xt[:], dxt, reads=[txt], writes=[d_["tx"][jj]])
                    if grp < 3 and NPAT == 4:
                        build_pat(grp * 4 + 4, 0)
                S.barrier()

        def phase_final():
            with ExitStack() as pes:
                def sb(name, shape, dt):
                    return pes.enter_context(nc.sbuf_tensor(uniq(name), list(shape), dt))
                fgrow = sb("fgrow", [128, D], F32)
                ss = sb("ss7", [128, 16], F32)
                rstd = sb("rstd7", [128, 16], F32)
                XT = sbring(pes, "xt7", 3, [128, D], F32, with_dsem=True)
                JK = sbring(pes, "jk7", 2, [128, D], F32)
                XO = sbring(pes, "xo7", 3, [128, D], F32, with_dsem=True)
                tl, tss = Tok(), Tok()
                S.dma(fgrow[:], fg_in.partition_broadcast(128), newd(), writes=[tl])
                S.dve("memset", ss[:], 0.0, writes=[tss])
                for jj in range(16):
                    j = 2 + jj
                    xt, txt, dxt = XT.next()
                    S.dma(xt[:], xs[j * 128:(j + 1) * 128, :], dxt, writes=[txt])
                    jk, tjk = JK.next()
                    tsj = Tok()
                    S.act(jk[:], xt[:], AF.Square, accum_out=ss[:, jj:jj + 1], reads=[txt, tss], writes=[tjk, tsj])
                    S.act(rstd[:, jj:jj + 1], ss[:, jj:jj + 1], AF.Ln, bias=epsc[:, 0:1], scale=1.0 / D,
                          reads=[tsj, tconst], writes=[tsj])
                    S.act(rstd[:, jj:jj + 1], rstd[:, jj:jj + 1], AF.Exp, scale=-0.5, reads=[tsj], writes=[tsj])
                    xo, txo, dxo = XO.next()
                    S.dve("scalar_tensor_tensor", xo[:], xt[:], rstd[:, jj:jj + 1], fgrow[:], ALU.mult, ALU.mult,
                          reads=[txt, tsj, tl], writes=[txo])
                    S.dma(out[jj * 128:(jj + 1) * 128, :], xo[:], dxo, reads=[txo])
                S.barrier()

        phases = []
        phases.append(("mod", phase_mod))
        for l in range(depth):
            need_ctx = l < depth - 1
            phases.append((f"inproj{l}", lambda l=l: phase_inproj(l)))
            phases.append((f"fourier{l}", lambda l=l: phase_fourier(l)))
            phases.append((f"attn{l}", lambda l=l: phase_attn(l)))
            phases.append((f"merge{l}", lambda l=l: phase_merge(l)))
            sets = [(2, 16, 256, 0)] + ([(0, 2, 32, 1)] if need_ctx else [])
            phases.append((f"moe{l}", lambda l=l, sets=sets: phase_moe(l, sets, dbgout=("dbg_aff" in dbg and l == 0))))
        for name, fn in phases:
            fn()
            if stop_after == name:
                break
        with nc.Block() as block:
            S.emit(block, sems)
    return nc


def _perm_cols():
    def heads(base, hs, swap):
        cols = []
        for h in hs:
            for d in range(64):
                cols.append(base + h * 64 + (d ^ 1 if swap else d))
        return cols
    cols = list(range(0, 256))
    GQ0, GK0, WQ0, WK0 = 256, 768, 1024, 1280
    for swap in (False, True):
        for c in range(4):
            cols += heads(GQ0, [c, 4 + c], swap)
    for swap in (False, True):
        cols += heads(GK0, [0, 1], swap)
    for swap in (False, True):
        cols += heads(WQ0, [0, 2], swap)
        cols += heads(WQ0, [1, 3], swap)
    for swap in (False, True):
        cols += heads(WK0, [0, 1], swap)
    cols += list(range(1536, 4608))
    return np.array(cols, dtype=np.int64)


def _consts():
    bf = ml_dtypes.bfloat16
    c = {}
    c["identf"] = np.eye(128, dtype=np.float32)
    c["identb"] = np.eye(128, dtype=np.float32).astype(bf)
    bdm = np.zeros((128, 128), np.float32)
    bdm[:64, :64] = 1
    bdm[64:, 64:] = 1
    c["bd"] = bdm.astype(bf)
    t = np.arange(T)
    r, col = t // 64, t % 64
    half = 32
    inv = 10000.0 ** (-np.arange(0, half, 2, dtype=np.float32) / half)
    ang = np.concatenate([r[:, None].astype(np.float32) * inv, col[:, None].astype(np.float32) * inv], axis=-1)
    cosv, sinv = np.cos(ang), np.sin(ang)
    cosT = np.zeros((128, T), np.float32)
    sinT = np.zeros((128, T), np.float32)
    for p in range(128):
        d = p % 64
        i = d // 2
        cosT[p] = cosv[:, i]
        sinT[p] = -sinv[:, i] if d % 2 == 0 else sinv[:, i]
    c["cos"], c["sin"] = cosT, sinT
    kj = np.arange(128)[:, None]
    qi = np.arange(128)[None, :]
    m0 = (kj >= qi).astype(np.float32)
    m1 = (kj <= qi).astype(np.float32)
    mk = np.zeros((128, 2, 256), np.float32)
    mk[:, 0, :128] = m0
    mk[:, 0, 128:] = m0
    mk[:, 1, :128] = m1
    mk[:, 1, 128:] = m1
    c["mk"] = mk.astype(bf)
    c["iota"] = np.tile(np.arange(256, dtype=np.float32)[None, :], (128, 1))
    c["pidx"] = np.stack([np.arange(128, dtype=np.float32), np.arange(128, dtype=np.float32) + 128], axis=1)
    sel = np.zeros((16, 16, 128), np.float32)
    for e in range(16):
        sel[e, e, :] = 1
    c["selb"] = sel.astype(bf)
    cc_, mm_ = np.meshgrid(np.arange(64), np.arange(64), indexing="ij")
    Cc = np.cos(2 * np.pi * cc_ * mm_ / 64) / 8.0
    Sc = np.sin(2 * np.pi * cc_ * mm_ / 64) / 8.0
    csc = np.zeros((128, 256), np.float64)
    for gblk in range(2):
        csc[gblk * 64:(gblk + 1) * 64, gblk * 64:(gblk + 1) * 64] = Cc
        csc[gblk * 64:(gblk + 1) * 64, 128 + gblk * 64:128 + (gblk + 1) * 64] = Sc
    c["csc"] = csc.astype(np.float32).astype(bf)

    def dft(N):
        n = np.arange(N)
        kn = (n[:, None] * n[None, :]) % N
        a = 2 * np.pi * kn / N
        return np.stack([np.cos(a) / np.sqrt(N), -np.sin(a) / np.sqrt(N)]).astype(np.float32).astype(bf)
    c["cs2048"] = dft(T)
    c["cs256"] = dft(L)
    return c


_CACHE = {}


def kernel(x, c, ctx, c_ctx, w_ada, b_ada, norm1_g, w_in, q_norm_g, k_norm_g, sink, w_br_fourier, w_br_global,
           w_br_window, w_out, norm2_g, w_router, w_gate_e, w_up_e, w_down_e, final_g, _dbg=(), _stop=None):
    f32 = np.float32
    x = np.asarray(x, f32)
    B = x.shape[0]
    key = (tuple(_dbg), _stop)
    if key not in _CACHE:
        _CACHE[key] = build(dbg=tuple(_dbg), stop_after=_stop)
    nc = _CACHE[key]
    consts = _consts()
    perm = _perm_cols()
    w_in = np.asarray(w_in, f32)
    w_inp = np.ascontiguousarray(w_in[:, :, perm])
    w_v = np.ascontiguousarray(np.concatenate([w_in[:, :, 896:1024], w_in[:, :, 1408:1536]], axis=2))
    b_ada = np.asarray(b_ada, f32)
    bcol = np.ascontiguousarray(b_ada.reshape(DEPTH, 48, 128).transpose(2, 0, 1))
    n1col = np.ascontiguousarray(np.asarray(norm1_g, f32).reshape(DEPTH, 8, 128).transpose(2, 0, 1))
    n2col = np.ascontiguousarray(np.asarray(norm2_g, f32).reshape(DEPTH, 8, 128).transpose(2, 0, 1))
    qg = np.asarray(q_norm_g, f32)
    kg = np.asarray(k_norm_g, f32)
    sw = np.arange(64) ^ 1
    qkg = np.zeros((128, DEPTH, 4), f32)
    for l in range(DEPTH):
        qkg[:, l, 0] = np.tile(qg[l], 2)
        qkg[:, l, 1] = np.tile(qg[l][sw], 2)
        qkg[:, l, 2] = np.tile(kg[l], 2)
        qkg[:, l, 3] = np.tile(kg[l][sw], 2)
    w_brc = np.ascontiguousarray(np.concatenate([np.asarray(w_br_fourier, f32), np.asarray(w_br_global, f32),
                                                 np.asarray(w_br_window, f32)], axis=1))
    shared = dict(
        w_ada=np.asarray(w_ada, f32), bcol=bcol, n1col=n1col, n2col=n2col, w_inp=w_inp, w_v=w_v, qkg=qkg,
        sink=np.asarray(sink, f32), w_br=w_brc, w_out=np.asarray(w_out, f32), w_r=np.asarray(w_router, f32),
        w_ge=np.asarray(w_gate_e, f32), w_ue=np.asarray(w_up_e, f32), w_de=np.asarray(w_down_e, f32),
        final_g=np.asarray(final_g, f32), **consts)
    c = np.asarray(c, f32)
    ctx = np.asarray(ctx, f32)
    c_ctx = np.asarray(c_ctx, f32)
    in_maps = []
    for b in range(B):
        cc = np.stack([c[b].reshape(8, 128).T, c_ctx.reshape(8, 128).T], axis=2)
        m = dict(shared)
        m["xin"] = np.ascontiguousarray(np.concatenate([ctx[b], x[b]], axis=0))
        m["ccin"] = np.ascontiguousarray(cc)
        in_maps.append(m)
    res = run_bass_kernel_spmd(nc, in_maps, core_ids=list(range(B)))
    if _dbg:
        return res.results
    return np.stack([r["out"] for r in res.results], axis=0).astype(f32)
```
